# Optimizing a Trainium2 kernel written in Bass

```python
import math
import jax
import jax.numpy as jnp
from jax import lax
import numpy as np

D_MODEL = 1024
BATCH = 8
SEQ = 8192
DEPTH = 2

CTX_LEN = 256
GRID_W = 64

S5_W = D_MODEL // 4
S5_GC = 16
S5_G = S5_W // S5_GC
S5_P = 64

MLA_H = D_MODEL // 128
MLA_NOPE = 64
MLA_ROPE = 32
MLA_V = 64
MLA_QR = D_MODEL // 4
MLA_KVR = D_MODEL // 8
MLA_W = MLA_H * MLA_V
MLA_SCALE = (MLA_NOPE + MLA_ROPE) ** -0.5
ROPE_AXIS = MLA_ROPE // 2
ROPE_BASE = 10000.0
Q_BLOCK = 128

ML_H = D_MODEL // 256
ML_D = 64
ML_W = ML_H * ML_D
ML_CHUNK = 64
ML_CONV = 3

MIX_W = S5_W + MLA_W + ML_W
IN_SIZES = (S5_W, MLA_QR, MLA_KVR, MLA_ROPE, ML_W, ML_W, ML_W, ML_W, 4 * ML_H)
N_IN = sum(IN_SIZES)

N_EXPERTS = 64
TOP_K = 8
N_EXPERT_GROUPS = 8
TOP_GROUPS = 4
EPG = N_EXPERTS // N_EXPERT_GROUPS
EXPERT_F = D_MODEL // 4
SHARED_F = D_MODEL // 4
ROUTED_SCALE = 2.5
ROUTE_BLOCK = 128

DEEPNORM_ALPHA = (2 * DEPTH) ** 0.25
DEEPNORM_BETA = (8 * DEPTH) ** -0.25
LN_EPS = 1e-5

kernel_name = 'hybrid_s5_mla_mlstm_moe_diffusion_trunk'


def _layer_norm(x, gain=None, bias=None):
    xf = x.astype(jnp.float32)
    mu = jnp.mean(xf, -1, keepdims=True)
    var = jnp.mean(jnp.square(xf - mu), -1, keepdims=True)
    y = (xf - mu) * lax.rsqrt(var + LN_EPS)
    if gain is not None:
        y = y * gain.astype(jnp.float32) + bias.astype(jnp.float32)
    return y.astype(x.dtype)


def _rms_norm(x, gain):
    xf = x.astype(jnp.float32)
    y = xf * lax.rsqrt(jnp.mean(xf * xf, -1, keepdims=True) + 1e-6) * gain.astype(jnp.float32)
    return y.astype(x.dtype)


def _split_cols(z):
    parts, off = [], 0
    for size in IN_SIZES:
        parts.append(z[..., off:off + size])
        off += size
    return parts


def _axial_angles(n_tokens):
    rows = n_tokens // GRID_W
    row_ids = jnp.repeat(jnp.arange(rows), GRID_W).astype(jnp.float32)
    col_ids = jnp.tile(jnp.arange(GRID_W), rows).astype(jnp.float32)
    half = ROPE_AXIS // 2
    inv_freq = ROPE_BASE ** (-jnp.arange(half, dtype=jnp.float32) / half)
    ang_r = row_ids[:, None] * inv_freq
    ang_c = col_ids[:, None] * inv_freq
    return jnp.cos(ang_r), jnp.sin(ang_r), jnp.cos(ang_c), jnp.sin(ang_c)


def _rot_half(v, cos, sin):
    v1, v2 = jnp.split(v, 2, axis=-1)
    return jnp.concatenate([v1 * cos - v2 * sin, v1 * sin + v2 * cos], axis=-1)


def _rope2d(v, cr, sr, cc, sc):
    vr, vc = jnp.split(v, 2, axis=-1)
    out = jnp.concatenate([_rot_half(vr, cr, sr), _rot_half(vc, cc, sc)], axis=-1)
    return out.astype(v.dtype)


def _linear_recurrence_combine(left, right):
    a_l, b_l = left
    a_r, b_r = right
    return a_r * a_l, a_r * b_l + b_r


def _s5_discretise(lam_re, lam_im, log_step, b_re, b_im, c_re, c_im):
    lam = lax.complex(lam_re.astype(jnp.float32), lam_im.astype(jnp.float32))
    step = jnp.exp(log_step.astype(jnp.float32))[..., None]
    a_bar = jnp.exp(lam * step)
    b_mat = lax.complex(b_re.astype(jnp.float32), b_im.astype(jnp.float32))
    b_bar = ((a_bar - 1.0) / lam)[..., None] * b_mat
    c_mat = lax.complex(c_re.astype(jnp.float32), c_im.astype(jnp.float32))
    return a_bar, b_bar, c_mat


def _s5_direction(ug, a_bar, b_bar, s0, reverse):
    bu = jnp.einsum('blgc,gpc->blgp', ug.astype(jnp.complex64), b_bar)
    if reverse:
        bu = bu[:, ::-1]
    bu = bu.at[:, 0].add(a_bar * s0)
    decay = jnp.broadcast_to(a_bar, (1,) + bu.shape[1:])
    _, states = lax.associative_scan(_linear_recurrence_combine, (decay, bu), axis=1)
    final = states[:, -1]
    if reverse:
        states = states[:, ::-1]
    return states, final


def _s5_readout(ug, st_f, st_b, c_mat, d_skip, glu_w, glu_b, dtype):
    y = (jnp.einsum('blgp,gcp->blgc', st_f, c_mat[0]).real
         + jnp.einsum('blgp,gcp->blgc', st_b, c_mat[1]).real
         + ug * d_skip.astype(jnp.float32).reshape(S5_G, S5_GC))
    y = y.reshape(y.shape[0], y.shape[1], S5_W)
    g = jax.nn.gelu(y)
    out = g * jax.nn.sigmoid(g @ glu_w.astype(jnp.float32) + glu_b.astype(jnp.float32))
    return out.astype(dtype)


def _s5_mixer(u_ctx, u_lat, lam_re, lam_im, log_step, b_re, b_im, c_re, c_im, d_skip, glu_w, glu_b, with_ctx_out):
    a_bar, b_bar, c_mat = _s5_discretise(lam_re, lam_im, log_step, b_re, b_im, c_re, c_im)
    bsz, n_ctx, _ = u_ctx.shape
    n_lat = u_lat.shape[1]
    zero = jnp.zeros((bsz, S5_G, S5_P), jnp.complex64)
    uc = u_ctx.astype(jnp.float32).reshape(bsz, n_ctx, S5_G, S5_GC)
    ul = u_lat.astype(jnp.float32).reshape(bsz, n_lat, S5_G, S5_GC)
    st_cf, fin_cf = _s5_direction(uc, a_bar[0], b_bar[0], zero, False)
    st_cb, fin_cb = _s5_direction(uc, a_bar[1], b_bar[1], zero, True)
    st_lf, _ = _s5_direction(ul, a_bar[0], b_bar[0], fin_cf, False)
    st_lb, _ = _s5_direction(ul, a_bar[1], b_bar[1], fin_cb, True)
    out_lat = _s5_readout(ul, st_lf, st_lb, c_mat, d_skip, glu_w, glu_b, u_lat.dtype)
    out_ctx = _s5_readout(uc, st_cf, st_cb, c_mat, d_skip, glu_w, glu_b, u_ctx.dtype) if with_ctx_out else None
    return out_lat, out_ctx


def _mla_queries(q_c, q_norm_g, w_q_up):
    q = _rms_norm(q_c, q_norm_g) @ w_q_up
    q = q.reshape(q.shape[0], q.shape[1], MLA_H, MLA_NOPE + MLA_ROPE)
    return q[..., :MLA_NOPE], q[..., MLA_NOPE:]


def _mla_keys_values(kv_c, kv_norm_g, w_kv_up):
    kv = _rms_norm(kv_c, kv_norm_g) @ w_kv_up
    kv = kv.reshape(kv.shape[0], kv.shape[1], MLA_H, MLA_NOPE + MLA_V)
    return kv[..., :MLA_NOPE], kv[..., MLA_NOPE:]


def _attend(q_nope, q_rope, k_nope, k_rope, v):
    s = (jnp.einsum('bqhd,bkhd->bhqk', q_nope, k_nope)
         + jnp.einsum('bqhr,bkr->bhqk', q_rope, k_rope))
    p = jax.nn.softmax(s.astype(jnp.float32) * MLA_SCALE, axis=-1).astype(v.dtype)
    return jnp.einsum('bhqk,bkhd->bqhd', p, v)


def _mla_mixer(qc_ctx, kvc_ctx, kr_ctx, qc_lat, kvc_lat, kr_lat, q_norm_g, w_q_up, kv_norm_g, w_kv_up, with_ctx_out):
    bsz, n_lat, _ = qc_lat.shape
    n_ctx = kvc_ctx.shape[1]
    cr, sr, cc, sc = _axial_angles(n_lat)
    kn_c, v_c = _mla_keys_values(kvc_ctx, kv_norm_g, w_kv_up)
    kn_l, v_l = _mla_keys_values(kvc_lat, kv_norm_g, w_kv_up)
    kr_l = _rope2d(kr_lat, cr, sr, cc, sc)
    qn_l, qr_l = _mla_queries(qc_lat, q_norm_g, w_q_up)
    qr_l = _rope2d(qr_l, cr[:, None], sr[:, None], cc[:, None], sc[:, None])
    k_nope = jnp.concatenate([kn_c, kn_l], axis=1)
    k_rope = jnp.concatenate([kr_ctx, kr_l], axis=1)
    v_all = jnp.concatenate([v_c, v_l], axis=1)
    n_blocks = n_lat // Q_BLOCK

    def to_blocks(t):
        return jnp.moveaxis(t.reshape(bsz, n_blocks, Q_BLOCK, *t.shape[2:]), 1, 0)

    o = lax.map(lambda qs: _attend(qs[0], qs[1], k_nope, k_rope, v_all), (to_blocks(qn_l), to_blocks(qr_l)))
    out_lat = jnp.moveaxis(o, 0, 1).reshape(bsz, n_lat, MLA_W)
    if with_ctx_out:
        qn_c, qr_c = _mla_queries(qc_ctx, q_norm_g, w_q_up)
        out_ctx = _attend(qn_c, qr_c, kn_c, kr_ctx, v_c).reshape(bsz, n_ctx, MLA_W)
    else:
        out_ctx = None
    return out_lat, out_ctx


def _centred_dwconv(x, w, b):
    n = x.shape[1]
    half = ML_CONV // 2
    xp = jnp.pad(x, ((0, 0), (half, half), (0, 0)))
    y = b
    for j in range(ML_CONV):
        y = y + xp[:, j:j + n] * w[j]
    return y


def _mlstm_inputs(q_in, k_in, v_in, g_in, conv_w, conv_b, gate_b):
    bsz, n, _ = q_in.shape
    qk = jax.nn.silu(_centred_dwconv(jnp.concatenate([q_in, k_in], -1), conv_w, conv_b).astype(jnp.float32))
    q = qk[..., :ML_W].reshape(bsz, n, ML_H, ML_D)
    k = qk[..., ML_W:].reshape(bsz, n, ML_H, ML_D) * (ML_D ** -0.5)
    v = v_in.astype(jnp.float32).reshape(bsz, n, ML_H, ML_D)
    g = (g_in.astype(jnp.float32) + gate_b.astype(jnp.float32).reshape(4 * ML_H)).reshape(bsz, n, 4, ML_H)
    fwd = (g[:, :, 0], jax.nn.log_sigmoid(g[:, :, 2]))
    bwd = (g[:, :, 1], jax.nn.log_sigmoid(g[:, :, 3]))
    return q, k, v, fwd, bwd


def _to_chunks(t):
    bsz, n = t.shape[:2]
    t = t.reshape(bsz, n // ML_CHUNK, ML_CHUNK, *t.shape[2:])
    return jnp.moveaxis(t, 3, 1)


def _mlstm_chunk_step(carry, inp):
    c_prev, n_prev, m_prev = carry
    c_loc, n_loc, m_loc, b_last = inp
    m_new = jnp.maximum(b_last + m_prev, m_loc)
    a = jnp.exp(b_last + m_prev - m_new)
    s = jnp.exp(m_loc - m_new)
    c_new = a[..., None, None] * c_prev + s[..., None, None] * c_loc
    n_new = a[..., None] * n_prev + s[..., None] * n_loc
    return (c_new, n_new, m_new), (c_prev, n_prev, m_prev)


def _mlstm_direction(q, k, v, log_i, log_f, state, reverse, with_out):
    if reverse:
        q, k, v, log_i, log_f = (t[:, ::-1] for t in (q, k, v, log_i, log_f))
    kc, vc, ic, fc = (_to_chunks(t) for t in (k, v, log_i, log_f))
    b = jnp.cumsum(fc, axis=-1)
    b_last = b[..., -1]
    w_end = b_last[..., None] - b + ic
    m_loc = jnp.max(w_end, axis=-1)
    e = jnp.exp(w_end - m_loc[..., None])
    c_loc = jnp.einsum('bhcs,bhcsd,bhcse->bhcde', e, vc, kc)
    n_loc = jnp.einsum('bhcs,bhcse->bhce', e, kc)
    final, incoming = lax.scan(_mlstm_chunk_step, state,
                               tuple(jnp.moveaxis(t, 2, 0) for t in (c_loc, n_loc, m_loc, b_last)))
    if not with_out:
        return None, final
    c_in, n_in, m_in = (jnp.moveaxis(t, 0, 2) for t in incoming)
    qc = _to_chunks(q)
    order = jnp.tril(jnp.ones((ML_CHUNK, ML_CHUNK), bool))
    log_d = jnp.where(order, b[..., :, None] - b[..., None, :] + ic[..., None, :], -jnp.inf)
    log_inter = b + m_in[..., None]
    m_t = jnp.maximum(log_inter, jnp.max(log_d, axis=-1))
    dw = jnp.exp(log_d - m_t[..., None])
    w_inter = jnp.exp(log_inter - m_t)
    s = jnp.einsum('bhctd,bhcsd->bhcts', qc, kc) * dw
    num = (jnp.einsum('bhcts,bhcsd->bhctd', s, vc)
           + w_inter[..., None] * jnp.einsum('bhcde,bhcte->bhctd', c_in, qc))
    den = jnp.sum(s, axis=-1) + w_inter * jnp.einsum('bhce,bhcte->bhct', n_in, qc)
    h = num / jnp.maximum(jnp.abs(den), jnp.exp(-m_t))[..., None]
    h = jnp.moveaxis(h, 1, 3).reshape(q.shape)
    if reverse:
        h = h[:, ::-1]
    return h, final


def _mlstm_output(h_f, h_b, o_in, norm_g, dtype):
    bsz, n = o_in.shape[:2]
    h = jax.nn.sigmoid(o_in.astype(jnp.float32)).reshape(bsz, n, ML_H, ML_D) * (h_f + h_b)
    mu = jnp.mean(h, -1, keepdims=True)
    var = jnp.mean(jnp.square(h - mu), -1, keepdims=True)
    h = ((h - mu) * lax.rsqrt(var + LN_EPS)).reshape(bsz, n, ML_W) * norm_g.astype(jnp.float32)
    return h.astype(dtype)


def _mlstm_mixer(parts_ctx, parts_lat, conv_w, conv_b, gate_b, norm_g, with_ctx_out):
    qc, kc, vc, g_fc, g_bc = _mlstm_inputs(parts_ctx[0], parts_ctx[1], parts_ctx[2], parts_ctx[4], conv_w, conv_b, gate_b)
    ql, kl, vl, g_fl, g_bl = _mlstm_inputs(parts_lat[0], parts_lat[1], parts_lat[2], parts_lat[4], conv_w, conv_b, gate_b)
    bsz = ql.shape[0]
    zero = (jnp.zeros((bsz, ML_H, ML_D, ML_D), jnp.float32),
            jnp.zeros((bsz, ML_H, ML_D), jnp.float32),
            jnp.zeros((bsz, ML_H), jnp.float32))
    h_cf, st_f = _mlstm_direction(qc, kc, vc, g_fc[0], g_fc[1], zero, False, with_ctx_out)
    h_cb, st_b = _mlstm_direction(qc, kc, vc, g_bc[0], g_bc[1], zero, True, with_ctx_out)
    h_lf, _ = _mlstm_direction(ql, kl, vl, g_fl[0], g_fl[1], st_f, False, True)
    h_lb, _ = _mlstm_direction(ql, kl, vl, g_bl[0], g_bl[1], st_b, True, True)
    out_lat = _mlstm_output(h_lf, h_lb, parts_lat[3], norm_g, parts_lat[3].dtype)
    out_ctx = _mlstm_output(h_cf, h_cb, parts_ctx[3], norm_g, parts_ctx[3].dtype) if with_ctx_out else None
    return out_lat, out_ctx


def _moe(h, router_w, router_bias, w_gate, w_up, w_down, sw_gate, sw_up, sw_down):
    n_tok = h.shape[0]
    scores = jax.nn.sigmoid((h @ router_w).astype(jnp.float32))
    biased = scores + router_bias.astype(jnp.float32)
    group_score = jnp.sum(lax.top_k(biased.reshape(n_tok, N_EXPERT_GROUPS, EPG), 2)[0], -1)
    kth = lax.top_k(group_score, TOP_GROUPS)[0][:, -1:]
    allowed = jnp.repeat(group_score >= kth, EPG, axis=1)
    _, top_e = lax.top_k(jnp.where(allowed, biased, -jnp.inf), TOP_K)
    gate = jnp.take_along_axis(scores, top_e, axis=1)
    gate = gate / jnp.sum(gate, -1, keepdims=True) * ROUTED_SCALE
    n_assign = n_tok * TOP_K
    e_flat = top_e.reshape(n_assign)
    tok_flat = jnp.repeat(jnp.arange(n_tok, dtype=jnp.int32), TOP_K)
    g_flat = gate.reshape(n_assign).astype(h.dtype)
    order = jnp.argsort(e_flat)
    e_sorted = e_flat[order]
    counts = jnp.bincount(e_flat, length=N_EXPERTS)
    starts = jnp.cumsum(counts) - counts
    padded = (counts + ROUTE_BLOCK - 1) // ROUTE_BLOCK * ROUTE_BLOCK
    pad_end = jnp.cumsum(padded)
    pad_start = pad_end - padded
    dest = pad_start[e_sorted] + jnp.arange(n_assign) - starts[e_sorted]
    n_rows = -(-(n_assign + N_EXPERTS * (ROUTE_BLOCK - 1)) // ROUTE_BLOCK) * ROUTE_BLOCK
    n_blocks = n_rows // ROUTE_BLOCK
    tok_rows = jnp.zeros((n_rows,), jnp.int32).at[dest].set(tok_flat[order])
    gate_rows = jnp.zeros((n_rows,), h.dtype).at[dest].set(g_flat[order])
    blk_expert = jnp.minimum(jnp.searchsorted(pad_end, jnp.arange(n_blocks) * ROUTE_BLOCK, side='right'), N_EXPERTS - 1)

    def expert_block(acc, blk):
        tok, g, e = blk
        xb = h[tok]
        ob = (jax.nn.silu(xb @ w_gate[e]) * (xb @ w_up[e])) @ w_down[e]
        return acc.at[tok].add(ob * g[:, None]), None

    routed, _ = lax.scan(expert_block, jnp.zeros_like(h),
                         (tok_rows.reshape(n_blocks, ROUTE_BLOCK), gate_rows.reshape(n_blocks, ROUTE_BLOCK), blk_expert))
    shared = (jax.nn.silu(h @ sw_gate) * (h @ sw_up)) @ sw_down
    return routed + shared


def setup_inputs(seed: int = 0) -> dict:
    key = jax.random.key(seed)
    ks = iter(jax.random.split(key, 48))
    f32 = jnp.float32
    L = DEPTH
    D = D_MODEL

    def nrm(shape, scale):
        return jax.random.normal(next(ks), shape, f32) * scale

    x = nrm((BATCH, SEQ, D), 1.0)
    c = nrm((BATCH, D), 1.0)
    ctx = nrm((BATCH, CTX_LEN, D), 1.0)
    c_ctx = nrm((D,), 1.0)
    ada_w = nrm((L, D, 6 * D), 0.5 * D ** -0.5)
    ada_b = nrm((L, 6 * D), 0.01)
    w_in = nrm((L, D, N_IN), D ** -0.5)
    s5_lambda_re = -0.5 + nrm((L, 2, S5_G, S5_P), 0.01)
    s5_lambda_im = math.pi * jnp.arange(S5_P, dtype=f32) + nrm((L, 2, S5_G, S5_P), 0.01)
    s5_log_step = jax.random.uniform(next(ks), (L, 2, S5_G), f32, math.log(1e-3), math.log(1e-1))
    s5_b_re = nrm((L, 2, S5_G, S5_P, S5_GC), (2 * S5_GC) ** -0.5)
    s5_b_im = nrm((L, 2, S5_G, S5_P, S5_GC), (2 * S5_GC) ** -0.5)
    s5_c_re = nrm((L, 2, S5_G, S5_GC, S5_P), (2 * S5_P) ** -0.5)
    s5_c_im = nrm((L, 2, S5_G, S5_GC, S5_P), (2 * S5_P) ** -0.5)
    s5_d = nrm((L, S5_W), 1.0)
    s5_glu_w = nrm((L, S5_W, S5_W), S5_W ** -0.5)
    s5_glu_b = nrm((L, S5_W), 0.01)
    mla_q_norm = 1.0 + nrm((L, MLA_QR), 0.01)
    mla_w_q_up = nrm((L, MLA_QR, MLA_H * (MLA_NOPE + MLA_ROPE)), MLA_QR ** -0.5)
    mla_kv_norm = 1.0 + nrm((L, MLA_KVR), 0.01)
    mla_w_kv_up = nrm((L, MLA_KVR, MLA_H * (MLA_NOPE + MLA_V)), MLA_KVR ** -0.5)
    ml_conv_w = nrm((L, ML_CONV, 2 * ML_W), ML_CONV ** -0.5)
    ml_conv_b = nrm((L, 2 * ML_W), 0.01)
    ml_i_bias = nrm((L, 2, ML_H), 0.1)
    ml_f_bias = jnp.linspace(3.0, 6.0, ML_H, dtype=f32) + nrm((L, 2, ML_H), 0.1)
    ml_gate_b = jnp.concatenate([ml_i_bias, ml_f_bias], axis=1)
    ml_norm_g = 1.0 + nrm((L, ML_W), 0.01)
    w_out = nrm((L, MIX_W, D), MIX_W ** -0.5 * DEEPNORM_BETA)
    ln1_g = 1.0 + nrm((L, D), 0.01)
    ln1_b = nrm((L, D), 0.01)
    ln2_g = 1.0 + nrm((L, D), 0.01)
    ln2_b = nrm((L, D), 0.01)
    router_w = nrm((L, D, N_EXPERTS), D ** -0.5)
    router_bias = nrm((L, N_EXPERTS), 0.01)
    exp_w_gate = nrm((L, N_EXPERTS, D, EXPERT_F), D ** -0.5)
    exp_w_up = nrm((L, N_EXPERTS, D, EXPERT_F), D ** -0.5)
    exp_w_down = nrm((L, N_EXPERTS, EXPERT_F, D), EXPERT_F ** -0.5 * DEEPNORM_BETA)
    sh_w_gate = nrm((L, D, SHARED_F), D ** -0.5)
    sh_w_up = nrm((L, D, SHARED_F), D ** -0.5)
    sh_w_down = nrm((L, SHARED_F, D), SHARED_F ** -0.5 * DEEPNORM_BETA)
    return {'x': x, 'c': c, 'ctx': ctx, 'c_ctx': c_ctx, 'ada_w': ada_w, 'ada_b': ada_b, 'w_in': w_in,
            's5_lambda_re': s5_lambda_re, 's5_lambda_im': s5_lambda_im, 's5_log_step': s5_log_step,
            's5_b_re': s5_b_re, 's5_b_im': s5_b_im, 's5_c_re': s5_c_re, 's5_c_im': s5_c_im, 's5_d': s5_d,
            's5_glu_w': s5_glu_w, 's5_glu_b': s5_glu_b, 'mla_q_norm': mla_q_norm, 'mla_w_q_up': mla_w_q_up,
            'mla_kv_norm': mla_kv_norm, 'mla_w_kv_up': mla_w_kv_up, 'ml_conv_w': ml_conv_w, 'ml_conv_b': ml_conv_b,
            'ml_gate_b': ml_gate_b, 'ml_norm_g': ml_norm_g, 'w_out': w_out, 'ln1_g': ln1_g, 'ln1_b': ln1_b,
            'ln2_g': ln2_g, 'ln2_b': ln2_b, 'router_w': router_w, 'router_bias': router_bias,
            'exp_w_gate': exp_w_gate, 'exp_w_up': exp_w_up, 'exp_w_down': exp_w_down,
            'sh_w_gate': sh_w_gate, 'sh_w_up': sh_w_up, 'sh_w_down': sh_w_down}


def reference(x, c, ctx, c_ctx, ada_w, ada_b, w_in, s5_lambda_re, s5_lambda_im, s5_log_step, s5_b_re, s5_b_im,
              s5_c_re, s5_c_im, s5_d, s5_glu_w, s5_glu_b, mla_q_norm, mla_w_q_up, mla_kv_norm, mla_w_kv_up,
              ml_conv_w, ml_conv_b, ml_gate_b, ml_norm_g, w_out, ln1_g, ln1_b, ln2_g, ln2_b, router_w, router_bias,
              exp_w_gate, exp_w_up, exp_w_down, sh_w_gate, sh_w_up, sh_w_down):
    x_lat, x_ctx = x, ctx
    d = x.shape[-1]
    n_lat_tok = x.shape[0] * x.shape[1]
    for layer in range(DEPTH):
        last = layer == DEPTH - 1
        mod_lat = jax.nn.silu(c) @ ada_w[layer] + ada_b[layer]
        mod_ctx = jax.nn.silu(c_ctx) @ ada_w[layer] + ada_b[layer]
        sh1, sc1, g1, sh2, sc2, g2 = jnp.split(mod_lat[:, None, :], 6, axis=-1)
        csh1, csc1, cg1, csh2, csc2, cg2 = jnp.split(mod_ctx, 6, axis=-1)

        h_lat = _layer_norm(x_lat) * (1 + sc1) + sh1
        h_ctx = _layer_norm(x_ctx) * (1 + csc1) + csh1
        z_lat = _split_cols(h_lat @ w_in[layer])
        z_ctx = _split_cols(h_ctx @ w_in[layer])
        s5_lat, s5_ctx = _s5_mixer(z_ctx[0], z_lat[0], s5_lambda_re[layer], s5_lambda_im[layer], s5_log_step[layer],
                                   s5_b_re[layer], s5_b_im[layer], s5_c_re[layer], s5_c_im[layer], s5_d[layer],
                                   s5_glu_w[layer], s5_glu_b[layer], not last)
        mla_lat, mla_ctx = _mla_mixer(z_ctx[1], z_ctx[2], z_ctx[3], z_lat[1], z_lat[2], z_lat[3],
                                      mla_q_norm[layer], mla_w_q_up[layer], mla_kv_norm[layer], mla_w_kv_up[layer],
                                      not last)
        ml_lat, ml_ctx = _mlstm_mixer(z_ctx[4:9], z_lat[4:9], ml_conv_w[layer], ml_conv_b[layer], ml_gate_b[layer],
                                      ml_norm_g[layer], not last)
        y_lat = jnp.concatenate([s5_lat, mla_lat, ml_lat], axis=-1) @ w_out[layer]
        x_lat = _layer_norm(DEEPNORM_ALPHA * x_lat + g1 * y_lat, ln1_g[layer], ln1_b[layer])

        f_lat = _layer_norm(x_lat) * (1 + sc2) + sh2
        if last:
            ffn_lat = _moe(f_lat.reshape(-1, d), router_w[layer], router_bias[layer], exp_w_gate[layer],
                           exp_w_up[layer], exp_w_down[layer], sh_w_gate[layer], sh_w_up[layer],
                           sh_w_down[layer]).reshape(x_lat.shape)
        else:
            y_ctx = jnp.concatenate([s5_ctx, mla_ctx, ml_ctx], axis=-1) @ w_out[layer]
            x_ctx = _layer_norm(DEEPNORM_ALPHA * x_ctx + cg1 * y_ctx, ln1_g[layer], ln1_b[layer])
            f_ctx = _layer_norm(x_ctx) * (1 + csc2) + csh2
            tokens = jnp.concatenate([f_lat.reshape(-1, d), f_ctx.reshape(-1, d)], axis=0)
            ffn = _moe(tokens, router_w[layer], router_bias[layer], exp_w_gate[layer], exp_w_up[layer],
                       exp_w_down[layer], sh_w_gate[layer], sh_w_up[layer], sh_w_down[layer])
            ffn_lat = ffn[:n_lat_tok].reshape(x_lat.shape)
            ffn_ctx = ffn[n_lat_tok:].reshape(x_ctx.shape)
            x_ctx = _layer_norm(DEEPNORM_ALPHA * x_ctx + cg2 * ffn_ctx, ln2_g[layer], ln2_b[layer])
        x_lat = _layer_norm(DEEPNORM_ALPHA * x_lat + g2 * ffn_lat, ln2_g[layer], ln2_b[layer])
    return x_lat
```

```python
import math
from contextlib import ExitStack

import numpy as np
import ml_dtypes
import concourse.bass as bass
import concourse.mybir as mybir
from concourse.bass_utils import run_bass_kernel_spmd

F32 = mybir.dt.float32
BF16 = mybir.dt.bfloat16
AF = mybir.ActivationFunctionType
ALU = mybir.AluOpType
AX = mybir.AxisListType

D = 1024
NCTX = 256
DEPTH = 2
N_IN = 1712
LN_EPS = 1e-5
ALPHA = (2 * DEPTH) ** 0.25
MLA_SCALE = 96 ** -0.5

ENGS = ('pe', 'dve', 'act', 'pool', 'sp')
SAME_SYNC = {'pe': False, 'dve': True, 'act': True, 'pool': True, 'sp': False}
DMA_RING = 8


class Dep:
    __slots__ = ('ws', 'r', 'rd', 'prevr')

    def __init__(self):
        self.ws = []
        self.r = {}
        self.rd = []
        self.prevr = []


class Ins:
    __slots__ = ('eng', 'fn', 'waits', 'signal', 'val', 'is_dma', 'didx')

    def __init__(self, eng, fn, is_dma=False):
        self.eng = eng
        self.fn = fn
        self.waits = []
        self.signal = False
        self.val = 0
        self.is_dma = is_dma
        self.didx = -1


class Sched:
    def __init__(self, nc, es):
        self.nc = nc
        self.lists = {e: [] for e in ENGS}
        self.sems = {e: es.enter_context(nc.semaphore('s_' + e)) for e in ENGS}
        self.rings = {q: [es.enter_context(nc.semaphore('d_%s%d' % (q, i))) for i in range(DMA_RING)]
                      for q in ('sp', 'pool', 'act')}
        self.dcount = {q: 0 for q in ('sp', 'pool', 'act')}
        self.dlist = {q: [] for q in ('sp', 'pool', 'act')}

    def _add(self, ins, R, W, Wm=()):
        ws = {}
        for d in R:
            for w in d.ws:
                ws[id(w)] = w
        for d in W:
            for w in d.ws:
                ws[id(w)] = w
            for r in d.r.values():
                ws[id(r)] = r
            for r in d.rd:
                ws[id(r)] = r
            for r in d.prevr:
                ws[id(r)] = r
        for d in Wm:
            if d.r or d.rd:
                d.prevr = list(d.r.values()) + list(d.rd) + list(d.ws)
                d.ws = []
                d.r = {}
                d.rd = []
            for r in d.prevr:
                ws[id(r)] = r
        for w in ws.values():
            if w is ins:
                continue
            if (not w.is_dma) and w.eng == ins.eng and not SAME_SYNC[ins.eng]:
                continue
            ins.waits.append(w)
        for d in R:
            if ins.is_dma:
                d.rd.append(ins)
            else:
                d.r[ins.eng] = ins
        for d in W:
            d.ws = [ins]
            d.r = {}
            d.rd = []
            d.prevr = []
        for d in Wm:
            d.ws.append(ins)
        self.lists[ins.eng].append(ins)
        return ins

    def op(self, eng, fn, R=(), W=(), Wm=()):
        return self._add(Ins(eng, fn), R, W, Wm)

    def dma(self, q, out, in_, R=(), W=(), Wm=(), **kw):
        ins = Ins(q, lambda e: e.dma_start(out=out, in_=in_, **kw), is_dma=True)
        ins.didx = self.dcount[q]
        self.dcount[q] += 1
        self.dlist[q].append(ins)
        return self._add(ins, R, W, Wm)

    def finalize(self):
        for e in ENGS:
            for ins in self.lists[e]:
                for w in ins.waits:
                    if not w.is_dma:
                        w.signal = True
        for e in ENGS:
            c = 0
            for ins in self.lists[e]:
                if ins.signal and not ins.is_dma:
                    c += 1
                    ins.val = c
        nc = self.nc

        def tok(w):
            if w.is_dma:
                return self.rings[w.eng][w.didx % DMA_RING], 16 * (w.didx // DMA_RING + 1)
            return self.sems[w.eng], w.val

        def run(ename, eng):
            seen = {}
            for ins in self.lists[ename]:
                waits = [tok(w) for w in ins.waits]
                if ins.is_dma and ins.didx >= DMA_RING:
                    waits.append(tok(self.dlist[ename][ins.didx - DMA_RING]))
                mx = {}
                for sem, val in waits:
                    k = id(sem)
                    if val > seen.get(k, 0) and val > mx.get(k, (None, 0))[1]:
                        mx[k] = (sem, val)
                for k, (sem, val) in mx.items():
                    eng.wait_ge(sem, val)
                    seen[k] = val
                if ins.fn is None:
                    continue
                r = ins.fn(eng)
                if ins.is_dma:
                    s, v = tok(ins)
                    r.then_inc(s, 16)
                elif ins.signal:
                    r.then_inc(self.sems[ename], 1)

        with nc.Block() as block:
            @block.tensor
            def _(e):
                run('pe', e)

            @block.vector
            def _(e):
                run('dve', e)

            @block.scalar
            def _(e):
                run('act', e)

            @block.gpsimd
            def _(e):
                run('pool', e)

            @block.sync
            def _(e):
                run('sp', e)


def _rope_tables(NL):
    T = NCTX + NL
    t = np.arange(NL)
    row = (t // 64).astype(np.float32)
    col = (t % 64).astype(np.float32)
    inv = (10000.0 ** (-np.arange(8, dtype=np.float32) / 8)).astype(np.float32)
    ar = row[None, :] * inv[:, None]
    ac = col[None, :] * inv[:, None]
    cos = np.ones((32, T), np.float32)
    sin = np.zeros((32, T), np.float32)
    cos[0:8, NCTX:] = np.cos(ar); cos[8:16, NCTX:] = np.cos(ar)
    cos[16:24, NCTX:] = np.cos(ac); cos[24:32, NCTX:] = np.cos(ac)
    sin[0:8, NCTX:] = np.sin(ar); sin[8:16, NCTX:] = np.sin(ar)
    sin[16:24, NCTX:] = np.sin(ac); sin[24:32, NCTX:] = np.sin(ac)
    return cos, sin


def host_consts(NL):
    cos, sin = _rope_tables(NL)
    ident = np.eye(128, dtype=np.float32)
    s = np.arange(128)
    tri_f = (s[:, None] <= s[None, :]).astype(np.float32)
    tri_b = (s[:, None] >= s[None, :]).astype(np.float32)
    sw = np.zeros((128, 128), np.float32)
    sw[np.arange(64), np.arange(64) + 64] = 1.0
    sw[np.arange(64) + 64, np.arange(64)] = 1.0
    blk = s // 16
    m8f = (blk[:, None] <= blk[None, :]).astype(np.float32)
    m8b = (blk[:, None] >= blk[None, :]).astype(np.float32)
    return {'k_cos': cos, 'k_sin': sin, 'k_ident': ident, 'k_trif': tri_f, 'k_trib': tri_b,
            'k_sw': sw, 'k_m8f': m8f, 'k_m8b': m8b}


class B:
    def __init__(self, NL, n_layers=DEPTH, debug=()):
        self.NL = NL
        self.T = NCTX + NL
        self.n_layers = n_layers
        self.debug = set(debug)
        self.nc = bass.Bass('TRN2', target_bir_lowering=False)
        self.es = ExitStack()
        self.S = Sched(self.nc, self.es)
        self.deps = {}
        self._n = 0
        self.outs = []

    def din(self, name, shape, dt=F32):
        return self.nc.dram_tensor(name, list(shape), dt, kind='ExternalInput').ap()

    def dscr(self, name, shape, dt=F32):
        kind = 'ExternalOutput' if name in self.debug else 'Internal'
        return self.nc.dram_tensor(name, list(shape), dt, kind=kind).ap()

    def sb(self, name, shape, dt=F32, persist=False):
        if not hasattr(self, 'arena'):
            self.arena = self.nc.alloc_sbuf_tensor('arena', [128, 51200], F32)
            self.a_lo = 0
            self.a_hi = 51200
        n = 1
        for d_ in shape[1:]:
            n *= d_
        words = n if dt == F32 else (n + 1) // 2
        words = (words + 7) // 8 * 8
        if persist:
            o = self.a_lo
            self.a_lo += words
        else:
            self.a_hi -= words
            o = self.a_hi
        assert self.a_lo <= self.a_hi, 'SBUF arena exhausted: %s' % name
        v = self.arena[:, o:o + (n if dt == F32 else (n + 1) // 2)]
        if dt != F32:
            v = v.bitcast(dt)
        if n % 2 and dt != F32:
            v = v[:, 0:n]
        P = shape[0]
        if len(shape) == 3:
            v = v.rearrange('p (a b) -> p a b', a=shape[1])
        elif len(shape) == 4:
            v = v.rearrange('p (a b c) -> p a b c', a=shape[1], b=shape[2])
        return v[0:P] if P < 128 else v

    def stage_begin(self):
        S = self.S
        lasts = []
        for e in ENGS:
            for ins in reversed(S.lists[e]):
                if ins.fn is not None and not ins.is_dma:
                    lasts.append(ins)
                    break
        for q in S.dlist:
            lasts.extend(S.dlist[q][-DMA_RING:])
        for e in ENGS:
            ins = Ins(e, None)
            ins.waits = [w for w in lasts if w.is_dma or w.eng != e]
            S.lists[e].append(ins)
        self.a_hi = 51200
        if hasattr(self, 'ln_st'):
            del self.ln_st

    def dep(self, name=None):
        d = Dep()
        return d

    def I(self, eng, method, R, W, *a, **kw):
        return self.S.op(eng, lambda e: getattr(e, method)(*a, **kw), R, W)

    def mm(self, out, lhsT, rhs, start, stop, R, W):
        return self.S.op('pe', lambda e: e.matmul(out, lhsT, rhs, start=start, stop=stop), R, W)

    def tp(self, out, in_, ident, R, W):
        return self.S.op('pe', lambda e: e.transpose(out, in_, ident), R, W)

    def act(self, out, in_, func, R, W, **kw):
        return self.S.op('act', lambda e: e.activation(out=out, in_=in_, func=func, **kw), R, W)

    def dma(self, q, out, in_, R, W, Wm=(), **kw):
        return self.S.dma(q, out, in_, R, W, Wm, **kw)

    def bank(self):
        i = self._n % 8
        self._n += 1
        return self.ps[i], self.dps[i]

    def setup(self):
        nc, NL, T = self.nc, self.NL, self.T
        L = DEPTH
        self.x = self.din('x', [NL, D])
        self.ctx = self.din('ctx', [NCTX, D])
        self.cvec = self.din('cvec', [2, D])
        shp = {
            'ada_w': [L, D, 6 * D], 'ada_b': [L, 6 * D], 'w_in': [L, D, N_IN],
            's5_lambda_re': [L, 2, 16, 64], 's5_lambda_im': [L, 2, 16, 64], 's5_log_step': [L, 2, 16],
            's5_b_re': [L, 2, 16, 64, 16], 's5_b_im': [L, 2, 16, 64, 16],
            's5_c_re': [L, 2, 16, 16, 64], 's5_c_im': [L, 2, 16, 16, 64], 's5_d': [L, 256],
            's5_glu_w': [L, 256, 256], 's5_glu_b': [L, 256], 'mla_q_norm': [L, 256],
            'mla_w_q_up': [L, 256, 768], 'mla_kv_norm': [L, 128], 'mla_w_kv_up': [L, 128, 1024],
            'ml_conv_w': [L, 3, 512], 'ml_conv_b': [L, 512], 'ml_gate_b': [L, 16], 'ml_norm_g': [L, 256],
            'w_out': [L, D, D], 'ln1_g': [L, D], 'ln1_b': [L, D], 'ln2_g': [L, D], 'ln2_b': [L, D],
            'router_w': [L, D, 64], 'router_bias': [L, 64],
            'exp_w_gate': [L, 64, D, 256], 'exp_w_up': [L, 64, D, 256], 'exp_w_down': [L, 64, 256, D],
            'sh_w_gate': [L, D, 256], 'sh_w_up': [L, D, 256], 'sh_w_down': [L, 256, D],
        }
        self.w = {k: self.din(k, v) for k, v in shp.items()}
        self.k = {'k_cos': self.din('k_cos', [32, T]), 'k_sin': self.din('k_sin', [32, T]),
                  'k_ident': self.din('k_ident', [128, 128]), 'k_trif': self.din('k_trif', [128, 128]),
                  'k_trib': self.din('k_trib', [128, 128]), 'k_sw': self.din('k_sw', [128, 128]),
                  'k_m8f': self.din('k_m8f', [128, 128]), 'k_m8b': self.din('k_m8b', [128, 128])}
        self.out = nc.dram_tensor('out', [NL, D], F32, kind='ExternalOutput').ap()
        self.xres = self.dscr('xres', [T, D])
        self.modv = self.dscr('modv', [2, 6 * D])
        self.zTu = self.dscr('zTu', [256, 8, T // 8], BF16)
        self.y8 = self.dscr('y8', [256, 8, T // 8])
        self.zTqk = self.dscr('zTqk', [512, T])
        self.QT = self.dscr('QT', [8, 96, T], BF16)
        self.KT = self.dscr('KT', [8, 96, T], BF16)
        self.Vv = self.dscr('Vv', [T, 512], BF16)
        self.mlV = self.dscr('mlV', [T, 256], BF16)
        self.mlO = self.dscr('mlO', [T, 256])
        self.mlG = self.dscr('mlG', [T, 16])
        self.catT = self.dscr('catT', [D, T], BF16)
        self.hfD = self.dscr('hfD', [T, 256])
        self.dd = {n: Dep() for n in ('xres', 'modv', 'zTu', 'zTqk', 'QT', 'KT', 'Vv', 'mlV', 'mlO', 'mlG',
                                      'catT', 'out', 'hfD', 'y8')}
        self.ps = [nc.alloc_psum_tensor('ps%d' % i, [128, 512], F32) for i in range(8)]
        self.dps = [Dep() for _ in range(8)]
        self.ident = self.sb('ident', [128, 128], persist=True); self.d_ident = Dep()
        self.identb = self.sb('identb', [128, 128], BF16, persist=True)
        self.dma('sp', self.ident[:], self.k['k_ident'], [], [self.d_ident])
        self.I('dve', 'tensor_copy', [self.d_ident], [self.d_ident], self.identb[:], self.ident[:])

    def finish(self, outs):
        ins = Ins('sp', None)
        ins.waits = list(outs)
        self.S.lists['sp'].append(ins)
        self.S.finalize()

    def stage_mod(self, l):
        nc = self.nc
        if not hasattr(self, 'modT'):
            self.modT = self.sb('modT', [128, 2, 6, 8], persist=True); self.d_modT = Dep()
            self.modbc = self.sb('modbc', [128, 2, 2, D], persist=True); self.d_modbc = Dep()
        self.cT = self.sb('cT', [128, 2, 8]); self.d_cT = Dep()
        self.adab = self.sb('adab', [2, 6 * D]); self.d_adab = Dep()
        self.modrow = self.sb('modrow', [2, 6 * D]); self.d_modrow = Dep()
        self.adaw = [self.sb('adaw%d' % i, [128, 8, 512]) for i in range(2)]
        self.d_adaw = [Dep(), Dep()]
        for r in range(2):
            self.dma('sp', self.cT[:, r, :], self.cvec[r].rearrange('(p kc) -> p kc', kc=8), [], [self.d_cT])
        self.act(self.cT[:], self.cT[:], AF.Silu, [self.d_cT], [self.d_cT])
        for r in range(2):
            self.dma('sp', self.adab[r:r + 1, :], self.w['ada_b'][l:l + 1, :], [], [self.d_adab])
        for nb in range(12):
            wt, dw = self.adaw[nb % 2], self.d_adaw[nb % 2]
            self.dma('sp', wt[:], self.w['ada_w'][l, :, nb * 512:(nb + 1) * 512].rearrange('(p kc) n -> p kc n', kc=8),
                     [], [dw])
            pb, dpb = self.bank()
            for kc in range(8):
                self.mm(pb[0:2, :], self.cT[:, :, kc], wt[:, kc, :], kc == 0, kc == 7, [self.d_cT, dw], [dpb])
            self.I('dve', 'tensor_tensor', [dpb, self.d_adab], [self.d_modrow],
                   self.modrow[:, nb * 512:(nb + 1) * 512], pb[0:2, :], self.adab[:, nb * 512:(nb + 1) * 512], ALU.add)
        o = self.dma('sp', self.modv, self.modrow[:], [self.d_modrow], [self.dd['modv']])
        for r in range(2):
            self.dma('sp', self.modT[:, r, :, :], self.modv[r].rearrange('(w kc p) -> p w kc', w=6, kc=8),
                     [self.dd['modv']], [self.d_modT], allow_slow_non_contiguous=True)
        for wq in (1, 4):
            self.I('dve', 'tensor_scalar', [self.d_modT], [self.d_modT], self.modT[:, :, wq, :], self.modT[:, :, wq, :],
                   1.0, None, ALU.add)
        for r in range(2):
            for gi, wq in enumerate((2, 5)):
                self.dma('sp', self.modbc[:, r, gi, :], self.modv[r:r + 1, wq * D:(wq + 1) * D].partition_broadcast(128),
                         [self.dd['modv']], [self.d_modbc])

    def ln_prep(self, xt, dx):
        if not hasattr(self, 'ln_st'):
            self.ln_st = self.sb('ln_st', [128, 2, 6]); self.d_lnst = Dep()
            self.ln_mv = self.sb('ln_mv', [128, 2]); self.ln_rs = self.sb('ln_rs', [128, 2])
            self.d_lnrs = Dep()
            self.ln_xh = self.sb('ln_xh', [128, D]); self.d_lnxh = Dep()
        st, mv, rs, xh = self.ln_st, self.ln_mv, self.ln_rs, self.ln_xh
        for j in range(2):
            self.I('dve', 'bn_stats', [dx], [self.d_lnst], st[:, j, :], xt[:, j * 512:(j + 1) * 512])
        self.I('dve', 'bn_aggr', [self.d_lnst], [self.d_lnrs], mv[:], st[:].rearrange('p a b -> p (a b)'))
        self.act(rs[:, 0:1], mv[:, 1:2], AF.Sqrt, [self.d_lnrs], [self.d_lnrs], bias=LN_EPS)
        self.I('dve', 'reciprocal', [self.d_lnrs], [self.d_lnrs], rs[:, 0:1], rs[:, 0:1])
        self.I('dve', 'scalar_tensor_tensor', [self.d_lnrs], [self.d_lnrs], rs[:, 1:2], mv[:, 0:1], -1.0, rs[:, 0:1],
               ALU.mult, ALU.mult)
        self.act(xh[:], xt[:], AF.Identity, [dx, self.d_lnrs], [self.d_lnxh], scale=rs[:, 0:1], bias=rs[:, 1:2])

    def ln_tp(self, r, w_shift, w_scale, hT, dhT, c0, hT32=None, banks=None):
        xh = self.ln_xh
        for half in range(2):
            pb, dpb = self.bank() if banks is None else banks[half % len(banks)]
            for q in range(4):
                kc = half * 4 + q
                self.tp(pb[:, q * 128:(q + 1) * 128], xh[:, kc * 128:(kc + 1) * 128], self.ident[:],
                        [self.d_lnxh, self.d_ident], [dpb])
            for q in range(4):
                kc = half * 4 + q
                self.I('dve', 'tensor_scalar', [dpb, self.d_modT], [dhT], hT[:, kc, c0:c0 + 128],
                       pb[:, q * 128:(q + 1) * 128], self.modT[:, r, w_scale, kc:kc + 1],
                       self.modT[:, r, w_shift, kc:kc + 1], ALU.mult, ALU.add)
                if hT32 is not None:
                    self.act(hT32[:, kc, 0:128], pb[:, q * 128:(q + 1) * 128], AF.Identity,
                             [dpb, self.d_modT], [dhT], scale=self.modT[:, r, w_scale, kc:kc + 1],
                             bias=self.modT[:, r, w_shift, kc:kc + 1])

    def ln_T(self, xt, dx, r, w_shift, w_scale, hT, dhT, c0, hT32=None):
        self.ln_prep(xt, dx)
        self.ln_tp(r, w_shift, w_scale, hT, dhT, c0, hT32)

    def macro_tiles(self):
        out = [(0, 2, 1)]
        t = NCTX
        while t < self.T:
            out.append((t, 4, 0))
            t += 512
        return out

    def src_rows(self, l, t0):
        if l == 0:
            if t0 < NCTX:
                return self.ctx[t0:t0 + 128, :]
            return self.x[t0 - NCTX:t0 - NCTX + 128, :]
        return self.xres[t0:t0 + 128, :]

    def stage_a_weights(self, l):
        if True:
            self.win = self.sb('win', [128, 8, N_IN], BF16); self.d_win = Dep()
            self.wkr = self.sb('wkr', [128, 8, 2, 96], BF16)
            self.wq = self.sb('wq', [128, 2, 768], BF16); self.d_wq = Dep()
            self.wqs = self.sb('wqs', [128, 2, 768], BF16)
            self.wkv = self.sb('wkv', [128, 1024], BF16); self.d_wkv = Dep()
            self.wkvV = self.sb('wkvV', [128, 512], BF16)
            self.qg = self.sb('qg', [128, 256]); self.kvg = self.sb('kvg', [128, 128]); self.d_g = Dep()
            self.gb = self.sb('gb', [128, 16])
        w = self.w
        self.dma('pool', self.win[:], w['w_in'][l].rearrange('(kc p) n -> p kc n', p=128), [], [self.d_win])
        self.dma('pool', self.wq[:], w['mla_w_q_up'][l].rearrange('(kc p) n -> p kc n', p=128), [], [self.d_wq])
        self.dma('pool', self.wkv[:], w['mla_w_kv_up'][l], [], [self.d_wkv])
        self.dma('sp', self.qg[:], w['mla_q_norm'][l:l + 1, :].partition_broadcast(128), [], [self.d_g])
        self.dma('sp', self.kvg[:], w['mla_kv_norm'][l:l + 1, :].partition_broadcast(128), [], [self.d_g])
        self.dma('sp', self.gb[:], w['ml_gate_b'][l:l + 1, :].partition_broadcast(128), [], [self.d_g])
        self.I('pool', 'memset', [], [self.d_win], self.wkr[:], 0.0)
        self.I('dve', 'memset', [], [self.d_wq], self.wqs[:], 0.0)
        self.I('dve', 'tensor_copy', [self.d_win], [self.d_win], self.wkr[:, :, 0, 64:96], self.win[:, :, 640:672])
        for a in range(2):
            o = 16 * a
            self.I('dve', 'tensor_scalar', [self.d_win], [self.d_win], self.wkr[:, :, 1, 64 + o:72 + o],
                   self.win[:, :, 648 + o:656 + o], -1.0, None, ALU.mult)
            self.I('dve', 'tensor_copy', [self.d_win], [self.d_win], self.wkr[:, :, 1, 72 + o:80 + o],
                   self.win[:, :, 640 + o:648 + o])
        for h in range(8):
            for a in range(2):
                o = h * 96 + 64 + 16 * a
                self.I('dve', 'tensor_scalar', [self.d_wq], [self.d_wq], self.wqs[:, :, o:o + 8],
                       self.wq[:, :, o + 8:o + 16], -1.0, None, ALU.mult)
                self.I('dve', 'tensor_copy', [self.d_wq], [self.d_wq], self.wqs[:, :, o + 8:o + 16],
                       self.wq[:, :, o:o + 8])
            self.I('dve', 'tensor_copy', [self.d_wkv], [self.d_wkv], self.wkvV[:, h * 64:(h + 1) * 64],
                   self.wkv[:, h * 128 + 64:h * 128 + 128])

    def stage_a(self, l):
        nc = self.nc
        if True:
            self.a_x = [self.sb('a_x%d' % i, [128, D]) for i in range(2)]; self.d_ax = [Dep(), Dep()]
            self.a_hT = self.sb('a_hT', [128, 8, 512], BF16); self.d_ahT = Dep()
            self.a_fm = [self.sb('a_fm%d' % i, [128, 512], BF16) for i in range(2)]; self.d_afm = [Dep(), Dep()]
            self.a_fm32 = [self.sb('a_fm32_%d' % i, [128, 512]) for i in range(2)]; self.d_afm32 = [Dep(), Dep()]
            self.a_cs = self.sb('a_cs', [96, 2, 512]); self.d_acs = Dep()
            self.a_rt = self.sb('a_rt', [96, 2, 512]); self.d_art = Dep()
            self.a_kr = self.sb('a_kr', [96, 512], BF16); self.d_akr = Dep()
            self.a_sq = self.sb('a_sq', [128, 256]); self.a_ss = self.sb('a_ss', [128, 4]); self.d_ass = Dep()
            self.a_qn = self.sb('a_qn', [128, 384], BF16); self.d_aqn = Dep()
            self.a_qnT = self.sb('a_qnT', [128, 3, 512], BF16); self.d_aqnT = Dep()
            self.a_gt = self.sb('a_gt', [128, 16]); self.a_ge = self.sb('a_ge', [128, 8]); self.d_agt = Dep()
            self.a_v = self.sb('a_v', [128, 256], BF16); self.d_av = Dep()
            self.a_o = self.sb('a_o', [128, 256]); self.d_ao = Dep()
            self.a_q = [self.sb('a_q%d' % i, [96, 512], BF16) for i in range(2)]; self.d_aq = [Dep(), Dep()]
            self.a_vv = self.sb('a_vv', [128, 512], BF16); self.d_avv = Dep()
        dd = self.dd
        win, dwin = self.win, self.d_win
        cnt = 0
        for (t0, nsub, r) in self.macro_tiles():
            N = nsub * 128
            hT, dhT = self.a_hT, self.d_ahT
            self.dma('sp', self.a_cs[64:96, 0, :N], self.k['k_cos'][:, t0:t0 + N], [], [self.d_acs])
            self.dma('sp', self.a_cs[64:96, 1, :N], self.k['k_sin'][:, t0:t0 + N], [], [self.d_acs])
            for s in range(nsub):
                xt, dx = self.a_x[s % 2], self.d_ax[s % 2]
                self.dma('sp', xt[:], self.src_rows(l, t0 + s * 128), [dd['xres']], [dx])
                self.ln_T(xt, dx, r, 0, 1, hT, dhT, s * 128)
            fm = [(0, 'u', 0), (128, 'u', 128), (672, 'qk', 0), (800, 'qk', 128), (928, 'qk', 256), (1056, 'qk', 384)]
            for (c0, kind, ro) in fm:
                pb, dpb = self.bank()
                for kc in range(8):
                    self.mm(pb[:, :N], win[:, kc, c0:c0 + 128], hT[:, kc, :N], kc == 0, kc == 7, [dwin, dhT], [dpb])
                cnt += 1
                if kind == 'u':
                    ft, dft = self.a_fm[cnt % 2], self.d_afm[cnt % 2]
                    self.I('dve', 'tensor_copy', [dpb], [dft], ft[:, :N].rearrange('p (s j) -> p s j', s=8),
                           pb[:, :N].rearrange('p (j s) -> p s j', s=8))
                    self.dma('sp', self.zTu[ro:ro + 128, :, t0 // 8:(t0 + N) // 8], ft[:, :N].rearrange('p (s j) -> p s j', s=8),
                             [dft], [], [dd['zTu']])
                else:
                    ft, dft = self.a_fm32[cnt % 2], self.d_afm32[cnt % 2]
                    self.act(ft[:, :N], pb[:, :N], AF.Copy, [dpb], [dft])
                    self.dma('sp', self.zTqk[ro:ro + 128, t0:t0 + N], ft[:, :N], [dft], [], [dd['zTqk']])
            pbs = []
            for v in range(2):
                pb, dpb = self.bank()
                for kc in range(8):
                    self.mm(pb[0:96, :N], self.wkr[:, kc, v, :], hT[:, kc, :N], kc == 0, kc == 7, [dwin, dhT], [dpb])
                pbs.append((pb, dpb))
            self.rope(pbs, N, self.a_kr, self.d_akr)
            for h in range(8):
                self.dma('sp', self.KT[h, 64:96, t0:t0 + N], self.a_kr[64:96, :N], [self.d_akr], [], [dd['KT']])
            for s in range(nsub):
                tt = t0 + s * 128
                cs = slice(s * 128, (s + 1) * 128)
                pa, dpa = self.bank()
                for kc in range(8):
                    self.mm(pa[:, 0:384], hT[:, kc, cs], win[:, kc, 256:640], kc == 0, kc == 7, [dwin, dhT], [dpa])
                for kc in range(8):
                    self.mm(pa[:, 384:400], hT[:, kc, cs], win[:, kc, 1696:1712], kc == 0, kc == 7, [dwin, dhT], [dpa])
                pbk, dpbk = self.bank()
                for kc in range(8):
                    self.mm(pbk[:, :], hT[:, kc, cs], win[:, kc, 1184:1696], kc == 0, kc == 7, [dwin, dhT], [dpbk])
                ss = self.a_ss
                self.act(self.a_sq[:, 0:256], pa[:, 0:256], AF.Square, [dpa], [self.d_ass], accum_out=ss[:, 0:1])
                self.act(self.a_sq[:, 0:128], pa[:, 256:384], AF.Square, [dpa, self.d_ass], [self.d_ass],
                         accum_out=ss[:, 1:2])
                self.act(ss[:, 2:3], ss[:, 0:1], AF.Sqrt, [self.d_ass], [self.d_ass], scale=1.0 / 256, bias=1e-6)
                self.act(ss[:, 3:4], ss[:, 1:2], AF.Sqrt, [self.d_ass], [self.d_ass], scale=1.0 / 128, bias=1e-6)
                self.I('dve', 'reciprocal', [self.d_ass], [self.d_ass], ss[:, 2:4], ss[:, 2:4])
                self.I('dve', 'scalar_tensor_tensor', [dpa, self.d_ass, self.d_g], [self.d_aqn], self.a_qn[:, 0:256],
                       pa[:, 0:256], ss[:, 2:3], self.qg[:], ALU.mult, ALU.mult)
                self.I('dve', 'scalar_tensor_tensor', [dpa, self.d_ass, self.d_g], [self.d_aqn], self.a_qn[:, 256:384],
                       pa[:, 256:384], ss[:, 3:4], self.kvg[:], ALU.mult, ALU.mult)
                pt, dpt = self.bank()
                ptb = pt[:].bitcast(BF16)
                for j in range(3):
                    self.tp(ptb[:, j * 128:(j + 1) * 128], self.a_qn[:, j * 128:(j + 1) * 128], self.identb[:],
                            [self.d_aqn, self.d_ident], [dpt])
                for j in range(3):
                    self.I('dve', 'tensor_copy', [dpt], [self.d_aqnT], self.a_qnT[:, j, cs], ptb[:, j * 128:(j + 1) * 128])
                gt, ge = self.a_gt, self.a_ge
                self.I('dve', 'tensor_tensor', [dpa, self.d_g], [self.d_agt], gt[:], pa[:, 384:400], self.gb[:], ALU.add)
                self.act(ge[:], gt[:, 8:16], AF.Exp, [self.d_agt], [self.d_agt], scale=-1.0)
                self.act(ge[:], ge[:], AF.Ln, [self.d_agt], [self.d_agt], bias=1.0)
                self.I('dve', 'tensor_scalar', [self.d_agt], [self.d_agt], gt[:, 8:16], ge[:], -1.0, None, ALU.mult)
                self.dma('sp', self.mlG[tt:tt + 128, :], gt[:], [self.d_agt], [], [dd['mlG']])
                self.I('dve', 'tensor_copy', [dpbk], [self.d_av], self.a_v[:], pbk[:, 0:256])
                self.dma('sp', self.mlV[tt:tt + 128, :], self.a_v[:], [self.d_av], [], [dd['mlV']])
                self.act(self.a_o[:], pbk[:, 256:512], AF.Sigmoid, [dpbk], [self.d_ao])
                self.dma('sp', self.mlO[tt:tt + 128, :], self.a_o[:], [self.d_ao], [], [dd['mlO']])
            qnT, dq = self.a_qnT, self.d_aqnT
            for h in range(8):
                pbs = []
                for v, wt in enumerate((self.wq, self.wqs)):
                    pb, dpb = self.bank()
                    for j in range(2):
                        self.mm(pb[0:96, :N], wt[:, j, h * 96:(h + 1) * 96], qnT[:, j, :N], j == 0, j == 1,
                                [self.d_wq, dq], [dpb])
                    pbs.append((pb, dpb))
                qt, dqt = self.a_q[h % 2], self.d_aq[h % 2]
                self.rope(pbs, N, qt, dqt)
                self.I('dve', 'tensor_copy', [pbs[0][1]], [dqt], qt[0:64, :N], pbs[0][0][0:64, :N])
                self.dma('sp', self.QT[h, :, t0:t0 + N], qt[:, :N], [dqt], [], [dd['QT']])
                pb, dpb = self.bank()
                self.mm(pb[0:64, :N], self.wkv[:, h * 128:h * 128 + 64], qnT[:, 2, :N], True, True, [self.d_wkv, dq], [dpb])
                kt, dkt = self.a_fm[h % 2], self.d_afm[h % 2]
                self.act(kt[0:64, :N], pb[0:64, :N], AF.Copy, [dpb], [dkt])
                self.dma('sp', self.KT[h, 0:64, t0:t0 + N], kt[0:64, :N], [dkt], [], [dd['KT']])
            for s in range(nsub):
                tt = t0 + s * 128
                pb, dpb = self.bank()
                self.mm(pb[:, :], qnT[:, 2, s * 128:(s + 1) * 128], self.wkvV[:], True, True, [dq, self.d_wkv], [dpb])
                self.I('dve', 'tensor_copy', [dpb], [self.d_avv], self.a_vv[:], pb[:, :])
                self.dma('sp', self.Vv[tt:tt + 128, :], self.a_vv[:], [self.d_avv], [], [dd['Vv']])

    def rope(self, pbs, N, out, dout):
        cs, rt = self.a_cs, self.a_rt
        (p0, d0), (p1, d1) = pbs
        self.I('dve', 'tensor_tensor', [d0, self.d_acs], [self.d_art], rt[64:96, 0, :N], p0[64:96, :N], cs[64:96, 0, :N],
               ALU.mult)
        self.I('dve', 'tensor_tensor', [d1, self.d_acs], [self.d_art], rt[64:96, 1, :N], p1[64:96, :N], cs[64:96, 1, :N],
               ALU.mult)
        self.I('dve', 'tensor_tensor', [self.d_art], [dout], out[64:96, :N], rt[64:96, 0, :N], rt[64:96, 1, :N], ALU.add)


    def stage_b(self, l):
        T, NL = self.T, self.NL
        nkc = T // 128
        if True:
            self.b_K = [self.sb('b_K%d' % i, [96, T], BF16) for i in range(2)]; self.d_bK = [Dep(), Dep()]
            self.b_V = [self.sb('b_V%d' % i, [128, nkc, 65], BF16) for i in range(2)]; self.d_bV = [Dep(), Dep()]
            self.b_Q = [self.sb('b_Q%d' % i, [96, 512], BF16) for i in range(2)]; self.d_bQ = [Dep(), Dep()]
            self.b_P = [self.sb('b_P%d' % i, [128, 512], BF16) for i in range(4)]; self.d_bP = [Dep() for _ in range(4)]
            self.b_E = self.sb('b_E', [65, 64]); self.d_bE = Dep()
            self.b_o = self.sb('b_o', [65, 512]); self.d_bo = Dep()
            self.b_rec = self.sb('b_rec', [64, 512]); self.d_brec = Dep()
            self.b_ob = [self.sb('b_ob%d' % i, [64, 512], BF16) for i in range(2)]; self.d_bob = [Dep(), Dep()]
            self.I('dve', 'memset', [], [self.d_bE], self.b_E[:], 0.0)
            self.I('dve', 'memset', [], [self.d_bE], self.b_E[64:65, :], 1.0)
            for i in range(2):
                self.I('dve', 'memset', [], [self.d_bV[i]], self.b_V[i][:, :, 64:65], 1.0)
        dd = self.dd
        ps, dps = self.ps, self.dps
        blocks = [(0, NCTX, 2)]
        t = NCTX
        while t < T:
            blocks.append((t, 512, nkc))
            t += 512
        si = 0
        bi = 0
        LA = 2
        for h in range(8):
            K, dK = self.b_K[h % 2], self.d_bK[h % 2]
            V, dV = self.b_V[h % 2], self.d_bV[h % 2]
            self.dma('sp', K[:], self.KT[h], [dd['KT']], [dK])
            self.dma('sp', V[:, :, 0:64], self.Vv[:, h * 64:(h + 1) * 64].rearrange('(kc p) c -> p kc c', p=128),
                     [dd['Vv']], [dV])
            items = []
            for (t0, N, nk) in blocks:
                for kc in range(nk):
                    items.append((bi, t0, N, kc, nk))
                bi += 1
            pend = {}
            for step in range(len(items) + LA):
                if step < len(items):
                    (b_, t0, N, kc, nk) = items[step]
                    Q, dQ = self.b_Q[b_ % 2], self.d_bQ[b_ % 2]
                    if kc == 0:
                        self.dma('sp', Q[:, :N], self.QT[h, :, t0:t0 + N], [dd['QT']], [dQ])
                    pb, dpb = ps[si % 4], dps[si % 4]
                    P, dP = self.b_P[si % 4], self.d_bP[si % 4]
                    si += 1
                    self.mm(pb[:, :N], K[:, kc * 128:(kc + 1) * 128], Q[:, :N], True, True, [dK, dQ], [dpb])
                    self.act(P[:, :N], pb[:, :N], AF.Exp, [dpb], [dP], scale=MLA_SCALE)
                    pend[step] = (P, dP)
                j = step - LA
                if j >= 0:
                    (b_, t0, N, kc, nk) = items[j]
                    P, dP = pend.pop(j)
                    po, dpo = ps[4 + b_ % 2], dps[4 + b_ % 2]
                    self.mm(po[0:65, :N], V[:, kc, :], P[:, :N], kc == 0, kc == nk - 1, [dV, dP], [dpo])
                    if kc == nk - 1:
                        self.I('dve', 'tensor_copy', [dpo], [self.d_bo], self.b_o[:, :N], po[0:65, :N])
                        pd, dpd = ps[6], dps[6]
                        self.mm(pd[0:64, :N], self.b_E[:], self.b_o[:, :N], True, True, [self.d_bE, self.d_bo], [dpd])
                        self.I('dve', 'reciprocal', [dpd], [self.d_brec], self.b_rec[:, :N], pd[0:64, :N])
                        ob, dob = self.b_ob[b_ % 2], self.d_bob[b_ % 2]
                        self.I('dve', 'tensor_tensor', [self.d_bo, self.d_brec], [dob], ob[:, :N], self.b_o[0:64, :N],
                               self.b_rec[:, :N], ALU.mult)
                        self.dma('sp', self.catT[256 + h * 64:256 + (h + 1) * 64, t0:t0 + N], ob[:, :N], [dob], [],
                                 [dd['catT']])

    def stage_c(self, l):
        T, NL = self.T, self.NL
        nch = T // 128
        dd = self.dd
        if True:
            self.c_x = self.sb('c_x', [128, T]); self.d_cx = Dep()
            self.c_acc = self.sb('c_acc', [128, T]); self.d_cacc = Dep()
            self.c_qk = self.sb('c_qk', [128, 4, T], BF16); self.d_cqk = Dep()
            self.c_cw = self.sb('c_cw', [128, 4, 4]); self.d_ccw = Dep()
            self.c_tri = self.sb('c_tri', [128, 2, 128]); self.d_ctri = Dep()
            self.c_trib = self.sb('c_trib', [128, 2, 128], BF16)
            self.c_ones = self.sb('c_ones', [128, 128])
            self.c_G = self.sb('c_G', [128, 16]); self.d_cG = Dep()
            self.c_gs = self.sb('c_gs', [128, 16]); self.d_cgs = Dep()
            self.c_al = self.sb('c_al', [128, 4]); self.d_cal = Dep()
            self.c_V = [self.sb('c_V%d' % i, [128, 4, 65], BF16) for i in range(2)]; self.d_cV = [Dep(), Dep()]
            self.c_P = [self.sb('c_P%d' % i, [128, 128], BF16) for i in range(2)]; self.d_cP = [Dep(), Dep()]
            self.c_kb = [self.sb('c_kb%d' % i, [128, 64], BF16) for i in range(2)]; self.d_ckb = [Dep(), Dep()]
            self.c_C32 = self.sb('c_C32', [128, 4, 65]); self.c_Cb = self.sb('c_Cb', [128, 4, 65], BF16)
            self.d_cC = [Dep() for _ in range(4)]
            self.c_La = self.sb('c_La', [128, 65]); self.d_cLa = Dep()
            self.c_hfw = [self.sb('c_hfw%d' % i, [128, 256]) for i in range(2)]; self.d_chfw = [Dep(), Dep()]
            self.c_hfr = self.sb('c_hfr', [128, 256]); self.d_chfr = Dep()
            self.c_h = self.sb('c_h', [128, 256]); self.d_ch = Dep()
            self.c_w = self.sb('c_w', [128, 8]); self.d_cw = Dep()
            self.c_O = self.sb('c_O', [128, 256]); self.d_cO = Dep()
            self.c_ng = self.sb('c_ng', [128, 256]); self.d_cng = Dep()
            self.c_st = self.sb('c_st', [128, 4, 6]); self.c_mv = self.sb('c_mv', [128, 4, 2]); self.d_cst = Dep()
            self.c_rs = self.sb('c_rs', [128, 4])
            self.c_hn = self.sb('c_hn', [128, 256], BF16); self.d_chn = Dep()
            self.c_hT = self.sb('c_hT', [128, 2, 128], BF16); self.d_chT = Dep()
            self.dma('sp', self.c_tri[:, 0, :], self.k['k_trif'], [], [self.d_ctri])
            self.dma('sp', self.c_tri[:, 1, :], self.k['k_trib'], [], [self.d_ctri])
            self.I('dve', 'tensor_copy', [self.d_ctri], [self.d_ctri], self.c_trib[:], self.c_tri[:])
            self.I('dve', 'memset', [], [self.d_ctri], self.c_ones[:], 1.0)
            for i in range(2):
                self.I('dve', 'memset', [], [self.d_cV[i]], self.c_V[i][:, :, 64:65], 1.0)
        w = self.w
        for c in range(4):
            for j in range(3):
                self.dma('sp', self.c_cw[:, c, j:j + 1], w['ml_conv_w'][l, j, c * 128:(c + 1) * 128].rearrange('(p o) -> p o', o=1),
                         [], [self.d_ccw], allow_slow_non_contiguous=True)
            self.dma('sp', self.c_cw[:, c, 3:4], w['ml_conv_b'][l, c * 128:(c + 1) * 128].rearrange('(p o) -> p o', o=1),
                     [], [self.d_ccw], allow_slow_non_contiguous=True)
        self.dma('sp', self.c_ng[:], w['ml_norm_g'][l:l + 1, :].partition_broadcast(128), [], [self.d_cng])
        x, acc, cw = self.c_x, self.c_acc, self.c_cw
        for c in range(4):
            self.dma('sp', x[:], self.zTqk[c * 128:(c + 1) * 128, :], [dd['zTqk']], [self.d_cx])
            self.I('dve', 'tensor_scalar', [self.d_cx, self.d_ccw], [self.d_cacc], acc[:], x[:], cw[:, c, 1:2], cw[:, c, 3:4],
                   ALU.mult, ALU.add)
            for (lo, hi) in ((1, NCTX), (NCTX + 1, T)):
                self.I('dve', 'scalar_tensor_tensor', [self.d_cx, self.d_ccw, self.d_cacc], [self.d_cacc], acc[:, lo:hi],
                       x[:, lo - 1:hi - 1], cw[:, c, 0:1], acc[:, lo:hi], ALU.mult, ALU.add)
            for (lo, hi) in ((0, NCTX - 1), (NCTX, T - 1)):
                self.I('dve', 'scalar_tensor_tensor', [self.d_cx, self.d_ccw, self.d_cacc], [self.d_cacc], acc[:, lo:hi],
                       x[:, lo + 1:hi + 1], cw[:, c, 2:3], acc[:, lo:hi], ALU.mult, ALU.add)
            self.act(acc[:], acc[:], AF.Silu, [self.d_cacc], [self.d_cacc])
            self.I('dve', 'tensor_scalar', [self.d_cacc], [self.d_cqk], self.c_qk[:, c, :], acc[:],
                   0.125 if c >= 2 else 1.0, None, ALU.mult)
        qk, dqk = self.c_qk, self.d_cqk
        orders = [list(range(nch)), [1, 0] + list(range(nch - 1, 1, -1))]
        Gt = [self.sb('c_Gt%d' % i, [128, 16]) for i in range(2)]; dGt = [Dep(), Dep()]
        gsb = [self.sb('c_gsb%d' % i, [128, 16]) for i in range(2)]; dgsb = [Dep(), Dep()]
        alb = [self.sb('c_alb%d' % i, [128, 4]) for i in range(2)]; dalb = [Dep(), Dep()]
        Vb = [self.sb('c_Vb%d' % i, [128, 4, 65], BF16) for i in range(3)]; dVb = [Dep(), Dep(), Dep()]
        Pb = [self.sb('c_Pb%d' % i, [128, 4, 128], BF16) for i in range(2)]; dPb = [Dep(), Dep()]
        Lab = [self.sb('c_Lab%d' % i, [128, 4, 65]) for i in range(2)]; dLab = [Dep(), Dep()]
        for i in range(3):
            self.I('dve', 'memset', [], [dVb[i]], Vb[i][:, :, 64:65], 1.0)
        ps, dps = self.ps, self.dps
        cnt = {'l': 0, 'k': 0}

        def local(d, j):
            li_ = cnt['l']
            cnt['l'] += 1
            cs = slice(j * 128, (j + 1) * 128)
            G, dG = Gt[li_ % 2], dGt[li_ % 2]
            gs, dgs = gsb[li_ % 2], dgsb[li_ % 2]
            al, dal = alb[li_ % 2], dalb[li_ % 2]
            V, dV = Vb[li_ % 3], dVb[li_ % 3]
            P, dP = Pb[li_ % 2], dPb[li_ % 2]
            La, dLa = Lab[li_ % 2], dLab[li_ % 2]
            self.dma('sp', G[:], self.mlG[cs, :], [dd['mlG']], [dG])
            self.dma('sp', V[:, :, 0:64], self.mlV[cs, :].rearrange('p (h c) -> p h c', h=4), [dd['mlV']], [dV])
            pg, dpg = ps[0], dps[0]
            self.mm(pg[:, 0:4], self.c_tri[:, d, :], G[:, 8 + 4 * d:12 + 4 * d], True, True, [self.d_ctri, dG], [dpg])
            self.mm(pg[:, 8:12], self.c_ones[:], G[:, 8 + 4 * d:12 + 4 * d], True, True, [self.d_ctri, dG], [dpg])
            self.I('dve', 'tensor_copy', [dpg], [dgs], gs[:, 0:4], pg[:, 0:4])
            self.I('dve', 'tensor_tensor', [dG, dgs], [dgs], gs[:, 4:8], G[:, 4 * d:4 * d + 4], gs[:, 0:4], ALU.subtract)
            self.act(gs[:, 4:8], gs[:, 4:8], AF.Exp, [dgs], [dgs])
            self.act(gs[:, 8:12], gs[:, 0:4], AF.Exp, [dgs], [dgs])
            self.act(al[:], pg[:, 8:12], AF.Exp, [dpg], [dal])
            pk, dpk = ps[1], dps[1]
            pkb = pk[:].bitcast(BF16)
            for c2 in range(2):
                self.tp(pkb[:, c2 * 128:(c2 + 1) * 128], qk[:, 2 + c2, cs], self.identb[:], [dqk, self.d_ident], [dpk])
            for h in range(4):
                c2, r0 = h // 2, (h % 2) * 64
                rr = slice(r0, r0 + 64)
                kT = qk[rr, 2 + c2, cs]
                qT = qk[rr, c2, cs]
                k_ = cnt['k']
                cnt['k'] += 1
                pS, dpS = ps[4 + k_ % 2], dps[4 + k_ % 2]
                pL, dpL = ps[6 + k_ % 2], dps[6 + k_ % 2]
                kb, dkb = self.c_kb[k_ % 2], self.d_ckb[k_ % 2]
                self.mm(pS[:, 0:128], kT, qT, True, True, [dqk], [dpS])
                self.I('dve', 'scalar_tensor_tensor', [dpS, dgs, self.d_ctri], [dP], P[:, h, :], pS[:, 0:128],
                       gs[:, 4 + h:5 + h], self.c_trib[:, d, :], ALU.mult, ALU.mult)
                self.I('dve', 'tensor_scalar', [dpk, dgs], [dkb], kb[:], pkb[:, h * 64:(h + 1) * 64],
                       gs[:, 4 + h:5 + h], None, ALU.mult)
                self.mm(pL[rr, 0:65], kb[:], V[:, h, :], True, True, [dkb, dV], [dpL])
                self.act(La[rr, h, :], pL[rr, 0:65], AF.Copy, [dpL, dal], [dLa], scale=al[rr, h:h + 1])
            return (d, j, gs, dgs, al, dal, V, dV, P, dP, La, dLa)

        def chain(loc, vi):
            (d, j, gs, dgs, al, dal, V, dV, P, dP, La, dLa) = loc
            cs = slice(j * 128, (j + 1) * 128)
            po, dpo = ps[2 + vi % 2], dps[2 + vi % 2]
            for h in range(4):
                c2, r0 = h // 2, (h % 2) * 64
                rr = slice(r0, r0 + 64)
                qT = qk[rr, c2, cs]
                self.mm(po[:, h * 65:(h + 1) * 65], P[:, h, :], V[:, h, :], True, False, [dP, dV], [dpo])
                self.mm(po[:, h * 65:(h + 1) * 65], qT, self.c_Cb[rr, h, :], False, True, [dqk, self.d_cC[h]], [dpo])
                self.I('dve', 'scalar_tensor_tensor', [dLa, dal, self.d_cC[h]], [self.d_cC[h]],
                       self.c_C32[rr, h, :], self.c_C32[rr, h, :], al[rr, h:h + 1], La[rr, h, :], ALU.mult, ALU.add)
                self.I('dve', 'tensor_copy', [self.d_cC[h]], [self.d_cC[h]], self.c_Cb[rr, h, :], self.c_C32[rr, h, :])
            wv = self.c_w
            pov = po[:, 0:260].rearrange('p (h c) -> p h c', h=4)
            self.I('dve', 'tensor_tensor', [dpo, dgs], [self.d_cw], wv[:, 0:4], pov[:, :, 64], gs[:, 8:12], ALU.mult)
            self.act(wv[:, 0:4], wv[:, 0:4], AF.Abs, [self.d_cw], [self.d_cw])
            self.I('dve', 'tensor_scalar', [self.d_cw], [self.d_cw], wv[:, 0:4], wv[:, 0:4], 1.0, None, ALU.max)
            self.I('dve', 'reciprocal', [self.d_cw], [self.d_cw], wv[:, 0:4], wv[:, 0:4])
            self.I('dve', 'tensor_tensor', [self.d_cw, dgs], [self.d_cw], wv[:, 4:8], wv[:, 0:4], gs[:, 8:12], ALU.mult)
            if d == 0:
                hw_, dhw = self.c_hfw[vi % 2], self.d_chfw[vi % 2]
                for h in range(4):
                    self.I('dve', 'tensor_scalar', [dpo, self.d_cw], [dhw], hw_[:, h * 64:(h + 1) * 64],
                           pov[:, h, 0:64], wv[:, 4 + h:5 + h], None, ALU.mult)
                self.dma('sp', self.hfD[cs, :], hw_[:], [dhw], [], [dd['hfD']])
            else:
                hh = self.c_h
                self.dma('sp', self.c_O[:], self.mlO[cs, :], [dd['mlO']], [self.d_cO])
                self.dma('sp', self.c_hfr[:], self.hfD[cs, :], [dd['hfD']], [self.d_chfr])
                for h in range(4):
                    self.I('dve', 'scalar_tensor_tensor', [dpo, self.d_cw, self.d_chfr], [self.d_ch],
                           hh[:, h * 64:(h + 1) * 64], pov[:, h, 0:64], wv[:, 4 + h:5 + h],
                           self.c_hfr[:, h * 64:(h + 1) * 64], ALU.mult, ALU.add)
                self.I('dve', 'tensor_tensor', [self.d_ch, self.d_cO], [self.d_ch], hh[:], hh[:], self.c_O[:], ALU.mult)
                for h in range(4):
                    self.I('dve', 'bn_stats', [self.d_ch], [self.d_cst], self.c_st[:, h, :], hh[:, h * 64:(h + 1) * 64])
                for h in range(4):
                    self.I('dve', 'bn_aggr', [self.d_cst], [self.d_cst], self.c_mv[:, h, :], self.c_st[:, h, :])
                self.act(self.c_rs[:], self.c_mv[:, :, 1], AF.Sqrt, [self.d_cst], [self.d_cst], bias=LN_EPS)
                self.I('dve', 'reciprocal', [self.d_cst], [self.d_cst], self.c_rs[:], self.c_rs[:])
                for h in range(4):
                    self.I('dve', 'tensor_scalar', [self.d_ch, self.d_cst], [self.d_ch], hh[:, h * 64:(h + 1) * 64],
                           hh[:, h * 64:(h + 1) * 64], self.c_mv[:, h, 0:1], self.c_rs[:, h:h + 1], ALU.subtract, ALU.mult)
                self.I('dve', 'tensor_tensor', [self.d_ch, self.d_cng], [self.d_chn], self.c_hn[:], hh[:], self.c_ng[:], ALU.mult)
                pt, dpt = ps[1], dps[1]
                ptb = pt[:].bitcast(BF16)
                for c2 in range(2):
                    self.tp(ptb[:, c2 * 128:(c2 + 1) * 128], self.c_hn[:, c2 * 128:(c2 + 1) * 128], self.identb[:],
                            [self.d_chn, self.d_ident], [dpt])
                self.I('dve', 'tensor_copy', [dpt], [self.d_chT], self.c_hT[:].rearrange('p a b -> p (a b)'), ptb[:, 0:256])
                for c2 in range(2):
                    self.dma('sp', self.catT[768 + c2 * 128:768 + (c2 + 1) * 128, cs], self.c_hT[:, c2, :], [self.d_chT], [],
                             [dd['catT']])

        vi = 0
        for d in range(2):
            for h in range(4):
                self.I('dve', 'memset', [], [self.d_cC[h]], self.c_C32[:, h, :], 0.0)
                self.I('dve', 'memset', [], [self.d_cC[h]], self.c_Cb[:, h, :], 0.0)
            od = orders[d]
            nxt = local(d, od[0])
            for i_, j in enumerate(od):
                cur = nxt
                if i_ + 1 < len(od):
                    nxt = local(d, od[i_ + 1])
                chain(cur, vi)
                vi += 1

    def ln_affine(self, xt, dx, gbc, bbc, out, dout):
        st, mv, rs = self.e_st, self.e_mv, self.e_rs
        for j in range(2):
            self.I('dve', 'bn_stats', [dx], [self.d_est], st[:, j, :], xt[:, j * 512:(j + 1) * 512])
        self.I('dve', 'bn_aggr', [self.d_est], [self.d_est], mv[:], st[:].rearrange('p a b -> p (a b)'))
        self.act(rs[:, 0:1], mv[:, 1:2], AF.Sqrt, [self.d_est], [self.d_est], bias=LN_EPS)
        self.I('dve', 'reciprocal', [self.d_est], [self.d_est], rs[:, 0:1], rs[:, 0:1])
        self.I('dve', 'scalar_tensor_tensor', [self.d_est], [self.d_est], rs[:, 1:2], mv[:, 0:1], -1.0, rs[:, 0:1],
               ALU.mult, ALU.mult)
        self.act(out[:], xt[:], AF.Identity, [dx, self.d_est], [dout], scale=rs[:, 0:1], bias=rs[:, 1:2])
        self.I('dve', 'tensor_tensor', [dout, self.d_ec], [dout], out[:], out[:], gbc, ALU.mult)
        self.I('dve', 'tensor_tensor', [dout, self.d_ec], [dout], out[:], out[:], bbc, ALU.add)

    def stage_e(self, l, last):
        T, NL = self.T, self.NL
        dd, w = self.dd, self.w
        ntile = T // 128
        SBT = 10
        self.e_st = self.sb('e_st', [128, 2, 6]); self.e_mv = self.sb('e_mv', [128, 2]); self.e_rs = self.sb('e_rs', [128, 2])
        self.d_est = Dep()
        wout = self.sb('e_wout', [128, 8, D], BF16); d_wout = Dep()
        lnc = self.sb('e_lnc', [128, 4, D]); self.d_ec = Dep()
        rw = self.sb('e_rw', [128, 8, 64]); rb = self.sb('e_rb', [128, 64]); d_rw = Dep()
        self.dma('pool', wout[:], w['w_out'][l].rearrange('(kc p) n -> p kc n', p=128), [], [d_wout])
        for i, nm in enumerate(('ln1_g', 'ln1_b', 'ln2_g', 'ln2_b')):
            self.dma('sp', lnc[:, i, :], w[nm][l:l + 1, :].partition_broadcast(128), [], [self.d_ec])
        self.dma('sp', rw[:], w['router_w'][l].rearrange('(kc p) n -> p kc n', p=128), [], [d_rw])
        self.dma('sp', rb[:], w['router_bias'][l:l + 1, :].partition_broadcast(128), [], [d_rw])
        cat = [self.sb('e_cat%d' % i, [128, 8, 128], BF16) for i in range(2)]; d_cat = [Dep(), Dep()]
        _x = self.sb('e_x0', [128, D]); _dx = Dep(); xt = [_x, _x]; d_xt = [_dx, _dx]
        tt_ = self.sb('e_t', [128, D]); d_t = Dep()
        _x1 = self.sb('e_x10', [128, D]); _dx1 = Dep(); x1 = [_x1, _x1]; d_x1 = [_dx1, _dx1]
        fTs = [self.sb('e_fT%d' % i, [128, 8, SBT * 128], BF16) for i in range(2)]; d_fTs = [Dep(), Dep()]
        fT32 = self.sb('e_fT32', [128, 8, 128]); d_fT32 = Dep()
        d_xh = Dep()
        acc = self.sb('e_acc', [128, SBT, D]); d_acc = [Dep() for _ in range(SBT)]
        Gs = [self.sb('e_G%d' % i, [128, SBT, 65]) for i in range(2)]; d_Gs = [Dep(), Dep()]
        rt = self.sb('e_rt', [128, 5, 64]); d_rt = Dep()
        t8 = self.sb('e_t8', [128, 9, 8]); d_t8 = Dep()
        sm = self.sb('e_sm', [128, 4, 8]); d_sm = Dep()
        wg = [self.sb('e_wg%d' % i, [128, 8, 512], BF16) for i in range(2)]
        wd = [self.sb('e_wd%d' % i, [128, 2, D], BF16) for i in range(2)]
        d_we = [Dep(), Dep()]
        hs = [self.sb('e_hs%d' % i, [128, 512]) for i in range(2)]; d_hs = [Dep(), Dep()]
        hid = [self.sb('e_hid%d' % i, [128, 2, 512], BF16) for i in range(2)]; d_hid = [Dep(), Dep()]
        self.xres1 = getattr(self, 'xres1', None) or self.dscr('xres1', [T, D])
        dd.setdefault('xres1', Dep())
        for i in range(2):
            self.I('dve', 'memset', [], [d_Gs[i]], Gs[i][:, :, 64:65], 1.0)
        ps, dps = self.ps, self.dps
        ei = 0
        hi_ = 0
        oi = 0
        tiles = list(range(ntile))
        sblocks = [tiles[i:i + SBT] for i in range(0, ntile, SBT)]
        P7 = (ps[7], dps[7])

        def p1_steps(k_sb):
            blk_ = sblocks[k_sb]
            fT, d_fT = fTs[k_sb % 2], d_fTs[k_sb % 2]
            G, d_G = Gs[k_sb % 2], d_Gs[k_sb % 2]
            steps = []
            for bi, ti in enumerate(blk_):
                t0 = ti * 128
                r = 1 if t0 < NCTX else 0
                ct, dct = cat[bi % 2], d_cat[bi % 2]
                xx, dxx = xt[bi % 2], d_xt[bi % 2]
                xo, dxo = x1[bi % 2], d_x1[bi % 2]

                def sA(t0=t0, r=r, ct=ct, dct=dct, xx=xx, dxx=dxx, xo=xo, dxo=dxo):
                    self.dma('sp', ct[:], self.catT[:, t0:t0 + 128].rearrange('(kc p) t -> p kc t', p=128), [dd['catT']], [dct])
                    self.dma('sp', xx[:], self.src_rows(l, t0), [dd['xres']], [dxx])
                    for half in range(2):
                        pb, dpb = P7
                        for kc in range(8):
                            self.mm(pb[:, :], ct[:, kc, :], wout[:, kc, half * 512:(half + 1) * 512], kc == 0, kc == 7,
                                    [dct, d_wout], [dpb])
                        self.I('dve', 'tensor_tensor', [dpb, self.d_modbc], [d_t], tt_[:, half * 512:(half + 1) * 512], pb[:, :],
                               self.modbc[:, r, 0, half * 512:(half + 1) * 512], ALU.mult)
                    self.I('dve', 'scalar_tensor_tensor', [dxx, d_t], [d_t], tt_[:], xx[:], ALPHA, tt_[:], ALU.mult, ALU.add)
                    self.ln_affine(tt_, d_t, lnc[:, 0, :], lnc[:, 1, :], xo, dxo)
                    self.dma('sp', self.xres1[t0:t0 + 128, :], xo[:], [dxo], [], [dd['xres1']])
                    self.ln_prep(xo, dxo)

                def sB(r=r, bi=bi):
                    self.ln_tp(r, 3, 4, fT, d_fT, bi * 128, hT32=fT32, banks=[P7])

                def sC(bi=bi):
                    pr, dpr = P7
                    for kc in range(8):
                        self.mm(pr[:, 0:64], fT32[:, kc, :], rw[:, kc, :], kc == 0, kc == 7, [d_fT, d_rw], [dpr])
                    self.act(rt[:, 0, :], pr[:, 0:64], AF.Sigmoid, [dpr], [d_rt])
                    self.I('dve', 'tensor_tensor', [d_rt, d_rw], [d_rt], rt[:, 1, :], rt[:, 0, :], rb[:], ALU.add)
                    for g in range(8):
                        self.I('dve', 'max', [d_rt], [d_t8], t8[:, g, :], rt[:, 1, g * 8:(g + 1) * 8])
                    self.I('dve', 'tensor_tensor', [d_t8], [d_sm], sm[:, 0, :], t8[:, 0:8, 0], t8[:, 0:8, 1], ALU.add)
                    self.I('dve', 'max', [d_sm], [d_t8], t8[:, 8, :], sm[:, 0, :])
                    self.I('dve', 'tensor_scalar', [d_sm, d_t8], [d_sm], sm[:, 1, :], sm[:, 0, :], t8[:, 8, 3:4], None, ALU.is_ge)
                    self.I('dve', 'tensor_scalar', [d_sm], [d_sm], sm[:, 2, :], sm[:, 1, :], -1.0, 1e9, ALU.add, ALU.mult)
                    for g in range(8):
                        self.I('dve', 'tensor_scalar', [d_rt, d_sm], [d_rt], rt[:, 2, g * 8:(g + 1) * 8],
                               rt[:, 1, g * 8:(g + 1) * 8], sm[:, 1, g:g + 1], sm[:, 2, g:g + 1], ALU.mult, ALU.add)
                    self.I('dve', 'max', [d_rt], [d_t8], t8[:, 8, :], rt[:, 2, :])
                    self.I('dve', 'tensor_scalar', [d_rt, d_t8], [d_rt], rt[:, 3, :], rt[:, 2, :], t8[:, 8, 7:8], None, ALU.is_ge)
                    self.I('dve', 'tensor_tensor', [d_rt], [d_rt], rt[:, 4, :], rt[:, 0, :], rt[:, 3, :], ALU.mult)
                    self.I('dve', 'reduce_sum', [d_rt], [d_sm], sm[:, 3, 0:1], rt[:, 4, :], AX.X)
                    self.I('dve', 'reciprocal', [d_sm], [d_sm], sm[:, 3, 0:1], sm[:, 3, 0:1])
                    self.I('dve', 'tensor_scalar', [d_rt, d_sm], [d_G], G[:, bi, 0:64], rt[:, 4, :], sm[:, 3, 0:1], 2.5,
                           ALU.mult, ALU.mult)
                steps += [sA, sB, sC]
            return steps

        for st_ in p1_steps(0):
            st_()
        for k_sb, blk in enumerate(sblocks):
            nb = len(blk)
            fT, d_fT = fTs[k_sb % 2], d_fTs[k_sb % 2]
            G, d_G = Gs[k_sb % 2], d_Gs[k_sb % 2]
            nxt_steps = p1_steps(k_sb + 1) if k_sb + 1 < len(sblocks) else []
            NB = nb * 128
            items = [(e, c0) for e in range(65) for c0 in range(0, NB, 512)]
            wbuf = {}

            def load_w(e):
                nonlocal ei
                wge, wde, dwe = wg[ei % 2], wd[ei % 2], d_we[ei % 2]
                ei += 1
                if e < 64:
                    srcs = (w['exp_w_gate'][l, e], w['exp_w_up'][l, e], w['exp_w_down'][l, e])
                else:
                    srcs = (w['sh_w_gate'][l], w['sh_w_up'][l], w['sh_w_down'][l])
                self.dma('pool', wge[:, :, 0:256], srcs[0].rearrange('(kc p) n -> p kc n', p=128), [], [dwe])
                self.dma('pool', wge[:, :, 256:512], srcs[1].rearrange('(kc p) n -> p kc n', p=128), [], [dwe])
                self.dma('pool', wde[:], srcs[2].rearrange('(fc p) n -> p fc n', p=128), [], [dwe])
                wbuf[e] = (wge, wde, dwe)

            def gate_up(e, c0):
                nonlocal hi_
                if e not in wbuf:
                    load_w(e)
                wge, wde, dwe = wbuf[e]
                N = min(512, NB - c0)
                hd, dhd = hid[hi_ % 2], d_hid[hi_ % 2]
                hi_ += 1
                for fc in range(2):
                    pg_, dpg_ = ps[2 * fc], dps[2 * fc]
                    pu_, dpu_ = ps[2 * fc + 1], dps[2 * fc + 1]
                    for kc in range(8):
                        self.mm(pg_[:, :N], wge[:, kc, fc * 128:(fc + 1) * 128], fT[:, kc, c0:c0 + N], kc == 0, kc == 7,
                                [dwe, d_fT], [dpg_])
                    for kc in range(8):
                        self.mm(pu_[:, :N], wge[:, kc, 256 + fc * 128:256 + (fc + 1) * 128], fT[:, kc, c0:c0 + N], kc == 0,
                                kc == 7, [dwe, d_fT], [dpu_])
                    hh, dhh = hs[fc], d_hs[fc]
                    self.act(hh[:, :N], pg_[:, :N], AF.Silu, [dpg_], [dhh])
                    self.I('dve', 'tensor_tensor', [dhh, dpu_], [dhd], hd[:, fc, :N], hh[:, :N], pu_[:, :N], ALU.mult)
                return hd, dhd

            def down(e, c0, hd, dhd):
                nonlocal oi
                wge, wde, dwe = wbuf[e]
                N = min(512, NB - c0)
                for s_ in range(N // 128):
                    bi = (c0 + s_ * 128) // 128
                    for half in range(2):
                        po_, dpo_ = ps[4 + oi % 3], dps[4 + oi % 3]
                        oi += 1
                        for fc in range(2):
                            self.mm(po_[:, :], hd[:, fc, s_ * 128:(s_ + 1) * 128], wde[:, fc, half * 512:(half + 1) * 512],
                                    fc == 0, fc == 1, [dhd, dwe], [dpo_])
                        av = acc[:, bi, half * 512:(half + 1) * 512]
                        if e == 0:
                            self.I('dve', 'tensor_scalar', [dpo_, d_G], [d_acc[bi]], av, po_[:, :], G[:, bi, e:e + 1], None,
                                   ALU.mult)
                        else:
                            self.I('dve', 'scalar_tensor_tensor', [dpo_, d_G, d_acc[bi]], [d_acc[bi]], av, po_[:, :],
                                   G[:, bi, e:e + 1], av, ALU.mult, ALU.add)

            prev = None
            n_done = 0
            for ii, (e, c0) in enumerate(items):
                want = (ii + 1) * len(nxt_steps) // len(items)
                if ii >= 2:
                    while n_done < want:
                        nxt_steps[n_done]()
                        n_done += 1
                hd, dhd = gate_up(e, c0)
                if prev is not None:
                    down(*prev)
                prev = (e, c0, hd, dhd)
                if c0 == 0 and e + 1 < 65 and (e + 1) not in wbuf:
                    load_w(e + 1)
                    wbuf.pop(e - 1, None)
            down(*prev)
            while n_done < len(nxt_steps):
                nxt_steps[n_done]()
                n_done += 1
            for bi, ti in enumerate(blk):
                t0 = ti * 128
                r = 1 if t0 < NCTX else 0
                xo, dxo = x1[bi % 2], d_x1[bi % 2]
                self.dma('sp', xo[:], self.xres1[t0:t0 + 128, :], [dd['xres1']], [dxo])
                self.I('dve', 'tensor_tensor', [d_acc[bi], self.d_modbc], [d_acc[bi]], acc[:, bi, :], acc[:, bi, :],
                       self.modbc[:, r, 1, :], ALU.mult)
                self.I('dve', 'scalar_tensor_tensor', [dxo, d_acc[bi]], [d_acc[bi]], acc[:, bi, :], xo[:], ALPHA, acc[:, bi, :],
                       ALU.mult, ALU.add)
                self.ln_affine(acc[:, bi, :], d_acc[bi], lnc[:, 2, :], lnc[:, 3, :], xo, dxo)
                if not last:
                    self.dma('sp', self.xres[t0:t0 + 128, :], xo[:], [dxo], [], [dd['xres']])
                elif t0 >= NCTX:
                    self.outs.append(self.dma('sp', self.out[t0 - NCTX:t0 - NCTX + 128, :], xo[:], [dxo], [], [dd['out']]))


    def stage_d(self, l):
        T, NL = self.T, self.NL
        NJ, NC8 = T // 8, NCTX // 8
        NL8 = NJ - NC8
        dd, w = self.dd, self.w
        dt_ = Dep()
        ops = {'n': 0}

        def V(eng, method, *a_, **kw):
            return self.I(eng, method, [dt_], [dt_], *a_, **kw)

        def A(out, in_, func, **kw):
            return self.act(out, in_, func, [dt_], [dt_], **kw)

        def tl(name, shape):
            return self.sb('d_' + name, shape)
        t1 = tl('t1', [128, 512]); t2 = tl('t2', [128, 512])

        def cm(or_, oi_, ar, ai, br, bi, n, shp=None):
            def v(t):
                x = t[:, 0:n]
                return x if shp is None else x.rearrange(shp[0], **shp[1])
            V('dve', 'tensor_tensor', v(t1), ar, br, ALU.mult)
            V('dve', 'tensor_tensor', v(t2), ai, bi, ALU.mult)
            V('dve', 'tensor_tensor', v(t1), v(t1), v(t2), ALU.subtract)
            V('dve', 'tensor_tensor', v(t2), ar, bi, ALU.mult)
            V('dve', 'tensor_tensor', oi_, ai, br, ALU.mult)
            V('dve', 'tensor_tensor', oi_, oi_, v(t2), ALU.add)
            V('dve', 'tensor_copy', or_, v(t1))

        lr = tl('lr', [128, 32]); li = tl('li', [128, 32]); dl = tl('dl', [128, 32])
        for hf in range(2):
            ps_ = slice(hf * 64, (hf + 1) * 64)
            self.dma('sp', lr[ps_, :], w['s5_lambda_re'][l].rearrange('d g p -> p (d g)'), [], [dt_], allow_slow_non_contiguous=True)
            self.dma('sp', li[ps_, :], w['s5_lambda_im'][l].rearrange('d g p -> p (d g)'), [], [dt_], allow_slow_non_contiguous=True)
        self.dma('sp', dl[:], w['s5_log_step'][l:l + 1].rearrange('o d g -> o (d g)').partition_broadcast(128), [], [dt_])
        A(dl[:], dl[:], AF.Exp)
        xr = tl('xr', [128, 32]); xi = tl('xi', [128, 32])
        V('dve', 'tensor_tensor', xr[:], lr[:], dl[:], ALU.mult)
        V('dve', 'tensor_tensor', xi[:], li[:], dl[:], ALU.mult)
        PWr = tl('PWr', [128, 16, 32]); PWi = tl('PWi', [128, 16, 32])
        NPr = tl('NPr', [128, 8, 32]); NPi = tl('NPi', [128, 8, 32])
        KK = max(1, int(math.ceil(math.log2(NJ))))
        AKr = tl('AKr', [128, KK, 32]); AKi = tl('AKi', [128, KK, 32])
        m16 = tl('m16', [128, 32]); c16 = tl('c16', [128, 32]); s16 = tl('s16', [128, 32])
        ar = tl('ar', [128, 32]); ai = tl('ai', [128, 32])
        A(m16[:], xr[:], AF.Exp, scale=1.0 / 16)
        A(c16[:], xi[:], AF.Sin, scale=1.0 / 16, bias=math.pi / 2)
        A(s16[:], xi[:], AF.Sin, scale=1.0 / 16)
        V('dve', 'tensor_tensor', ar[:], m16[:], c16[:], ALU.mult)
        V('dve', 'tensor_tensor', ai[:], m16[:], s16[:], ALU.mult)
        for _ in range(4):
            cm(ar[:], ai[:], ar[:], ai[:], ar[:], ai[:], 32)
        V('dve', 'memset', PWr[:, 0, :], 1.0); V('dve', 'memset', PWi[:, 0, :], 0.0)
        for s_ in range(1, 16):
            cm(PWr[:, s_, :], PWi[:, s_, :], PWr[:, s_ - 1, :], PWi[:, s_ - 1, :], ar[:], ai[:], 32)
        iar = tl('iar', [128, 32]); iai = tl('iai', [128, 32]); n2 = tl('n2', [128, 32])
        V('dve', 'tensor_tensor', n2[:], ar[:], ar[:], ALU.mult)
        V('dve', 'tensor_tensor', iar[:], ai[:], ai[:], ALU.mult)
        V('dve', 'tensor_tensor', n2[:], n2[:], iar[:], ALU.add)
        V('dve', 'reciprocal', n2[:], n2[:])
        V('dve', 'tensor_tensor', iar[:], ar[:], n2[:], ALU.mult)
        V('dve', 'scalar_tensor_tensor', iai[:], ai[:], -1.0, n2[:], ALU.mult, ALU.mult)
        V('dve', 'memset', NPr[:, 0, :], 1.0); V('dve', 'memset', NPi[:, 0, :], 0.0)
        for s_ in range(1, 8):
            cm(NPr[:, s_, :], NPi[:, s_, :], NPr[:, s_ - 1, :], NPi[:, s_ - 1, :], iar[:], iai[:], 32)
        V('dve', 'tensor_copy', AKr[:, 0, :], PWr[:, 8, :]); V('dve', 'tensor_copy', AKi[:, 0, :], PWi[:, 8, :])
        for k in range(1, KK):
            cm(AKr[:, k, :], AKi[:, k, :], AKr[:, k - 1, :], AKi[:, k - 1, :], AKr[:, k - 1, :], AKi[:, k - 1, :], 32)
        V('dve', 'tensor_scalar', AKi[64:128, :, :], AKi[64:128, :, :], -1.0, None, ALU.mult)
        cr = tl('cr', [128, 32]); ci = tl('ci', [128, 32]); am = tl('am', [128, 32]); dn = tl('dn', [128, 32])
        V('dve', 'tensor_scalar', am[:], ar[:], -1.0, None, ALU.add)
        V('dve', 'tensor_tensor', cr[:], am[:], lr[:], ALU.mult)
        V('dve', 'tensor_tensor', dn[:], ai[:], li[:], ALU.mult)
        V('dve', 'tensor_tensor', cr[:], cr[:], dn[:], ALU.add)
        V('dve', 'tensor_tensor', ci[:], ai[:], lr[:], ALU.mult)
        V('dve', 'tensor_tensor', dn[:], am[:], li[:], ALU.mult)
        V('dve', 'tensor_tensor', ci[:], ci[:], dn[:], ALU.subtract)
        V('dve', 'tensor_tensor', dn[:], lr[:], lr[:], ALU.mult)
        V('dve', 'tensor_tensor', am[:], li[:], li[:], ALU.mult)
        V('dve', 'tensor_tensor', dn[:], dn[:], am[:], ALU.add)
        V('dve', 'reciprocal', dn[:], dn[:])
        V('dve', 'tensor_tensor', cr[:], cr[:], dn[:], ALU.mult)
        V('dve', 'tensor_tensor', ci[:], ci[:], dn[:], ALU.mult)
        Br = tl('Br', [128, 32, 16]); Bi = tl('Bi', [128, 32, 16])
        for hf in range(2):
            ps_ = slice(hf * 64, (hf + 1) * 64)
            self.dma('sp', Br[ps_], w['s5_b_re'][l].rearrange('d g p c -> p (d g) c'), [], [dt_])
            self.dma('sp', Bi[ps_], w['s5_b_im'][l].rearrange('d g p c -> p (d g) c'), [], [dt_])
        shp3 = ('p (a b) -> p a b', {'a': 32})
        bc3 = lambda t: t.unsqueeze(2).to_broadcast([128, 32, 16])
        cm(Br[:], Bi[:], bc3(cr[:]), bc3(ci[:]), Br[:], Bi[:], 512, shp3)
        Cr = tl('Cr', [128, 32, 16]); Ci = tl('Ci', [128, 32, 16])
        cn = tl('cn', [128, 128])
        idn = self.ident
        for (nm, Ct) in (('s5_c_re', Cr), ('s5_c_im', Ci)):
            for d in range(2):
                for gh in range(2):
                    src = w[nm][l, d, gh * 8:(gh + 1) * 8].rearrange('g c p -> (g c) p')
                    self.dma('sp', cn[:, 0:64], src, [], [dt_])
                    self.dma('sp', cn[:, 64:128], src, [], [dt_])
                    pb, dpb = self.ps[0], self.dps[0]
                    self.tp(pb[:, 0:128], cn[:], idn[:], [dt_, self.d_ident], [dpb])
                    o0 = d * 16 + gh * 8
                    self.I('dve', 'tensor_copy', [dpb, dt_], [dt_], Ct[:, o0:o0 + 8, :].rearrange('p a b -> p (a b)'), pb[:, 0:128])
        XS = tl('XS', [128, 32, 8, 16]); XSA = tl('XSA', [128, 32, 8, 16]); YS = tl('YS', [128, 32, 8, 16])
        bc2 = lambda t: t.unsqueeze(2).to_broadcast([t.shape[0], 16, 16])
        h0, h1 = slice(0, 64), slice(64, 128)
        t1v = t1[:, 0:256].rearrange('p (a b) -> p a b', a=16)
        t2v = t2[:, 0:256].rearrange('p (a b) -> p a b', a=16)

        def cmh(out, pr, pi_, br, bi, neg):
            V('dve', 'tensor_tensor', t1v[h0], bc2(pr[h0]), br[h0], ALU.mult)
            V('dve', 'tensor_tensor', t2v[h0], bc2(pi_[h0]), bi[h0], ALU.mult)
            V('dve', 'tensor_tensor', out[h0], t1v[h0], t2v[h0], ALU.subtract)
            V('dve', 'tensor_tensor', t1v[h1], bc2(pr[h1]), bi[h1], ALU.mult)
            V('dve', 'tensor_tensor', t2v[h1], bc2(pi_[h1]), br[h1], ALU.mult)
            if neg:
                V('dve', 'scalar_tensor_tensor', out[h1], t1v[h1], -1.0, t2v[h1], ALU.mult, ALU.subtract)
            else:
                V('dve', 'tensor_tensor', out[h1], t1v[h1], t2v[h1], ALU.add)
        for d in range(2):
            dc = slice(d * 16, (d + 1) * 16)
            for s_ in range(8):
                if d == 0:
                    pX = (NPr[:, s_, dc], NPi[:, s_, dc]); pXA = (PWr[:, 8 - s_, dc], PWi[:, 8 - s_, dc])
                    pY = (PWr[:, s_, dc], PWi[:, s_, dc])
                else:
                    pX = (PWr[:, s_, dc], PWi[:, s_, dc]); pXA = (PWr[:, 8 + s_, dc], PWi[:, 8 + s_, dc])
                    pY = (NPr[:, s_, dc], NPi[:, s_, dc])
                cmh(XS[:, dc, s_, :], pX[0], pX[1], Br[:, dc, :], Bi[:, dc, :], False)
                cmh(XSA[:, dc, s_, :], pXA[0], pXA[1], Br[:, dc, :], Bi[:, dc, :], False)
                cmh(YS[:, dc, s_, :], pY[0], pY[1], Cr[:, dc, :], Ci[:, dc, :], True)
        YSb = self.sb('d_YSb', [128, 32, 128], BF16)
        V('dve', 'tensor_copy', YSb[:], YS[:].rearrange('p a b c -> p a (b c)'))
        XTAb = self.sb('d_XTAb', [128, 32, 128], BF16)
        KMb = self.sb('d_KMb', [128, 16, 128], BF16)
        cst = tl('cst', [128, 3, 128])
        self.dma('sp', cst[:, 0, :], self.k['k_sw'], [], [dt_])
        self.dma('sp', cst[:, 1, :], self.k['k_m8f'], [], [dt_])
        self.dma('sp', cst[:, 2, :], self.k['k_m8b'], [], [dt_])
        dS = tl('dS', [128, 16])
        for s_ in range(8):
            self.dma('sp', dS[16 * s_:16 * s_ + 16, :], w['s5_d'][l].rearrange('(g c) -> c g', c=16), [], [dt_],
                     allow_slow_non_contiguous=True)
        km = tl('km', [128, 128])
        for dg in range(32):
            pb, dpb = self.ps[dg % 2], self.dps[dg % 2]
            self.tp(pb[:, 0:128], XSA[:, dg, :, :].rearrange('p b c -> p (b c)'), idn[:], [dt_, self.d_ident], [dpb])
            self.I('dve', 'tensor_copy', [dpb, dt_], [dt_], XTAb[:, dg, :], pb[:, 0:128])
        for g in range(16):
            pf, dpf = self.ps[2], self.dps[2]
            pb_, dpb_ = self.ps[3], self.dps[3]
            self.mm(pf[:, 0:128], XS[:, g, :, :].rearrange('p b c -> p (b c)'), YS[:, g, :, :].rearrange('p b c -> p (b c)'),
                    True, True, [dt_], [dpf])
            self.mm(pb_[:, 0:128], XS[:, 16 + g, :, :].rearrange('p b c -> p (b c)'),
                    YS[:, 16 + g, :, :].rearrange('p b c -> p (b c)'), True, True, [dt_], [dpb_])
            self.I('dve', 'tensor_tensor', [dpf, dt_], [dt_], km[:], pf[:, 0:128], cst[:, 1, :], ALU.mult)
            self.I('dve', 'tensor_tensor', [dpb_, dt_], [dt_], t1[:, 0:128], pb_[:, 0:128], cst[:, 2, :], ALU.mult)
            V('dve', 'tensor_tensor', km[:], km[:], t1[:, 0:128], ALU.add)
            V('dve', 'scalar_tensor_tensor', KMb[:, g, :], idn[:], dS[:, g:g + 1], km[:], ALU.mult, ALU.add)
        U = [self.sb('d_U%d' % i, [128, NJ], BF16) for i in range(2)]; dU = [Dep(), Dep()]
        P = [self.sb('d_P%d' % i, [128, NJ]) for i in range(2)]; dP = [Dep(), Dep()]
        Sx = [self.sb('d_S%d' % i, [128, NJ], BF16) for i in range(2)]; dSx = [Dep(), Dep()]
        Rm = [self.sb('d_R%d' % i, [128, 128]) for i in range(4)]; dR = [Dep() for _ in range(4)]
        yg = [self.sb('d_yg%d' % i, [128, NJ]) for i in range(2)]; dyg = [Dep(), Dep()]
        blocks = [(c0, min(512, NJ - c0)) for c0 in range(0, NJ, 512)]
        ps, dps = self.ps, self.dps
        ri = 0
        for g in range(16):
            Ug, dUg = U[g % 2], dU[g % 2]
            for s_ in range(8):
                self.dma('sp', Ug[16 * s_:16 * s_ + 16, :], self.zTu[16 * g:16 * g + 16, s_, :], [dd['zTu']], [dUg])
            for d in range(2):
                dg = d * 16 + g
                Pd, dPd = P[d], dP[d]
                for (c0, n) in blocks:
                    pb, dpb = ps[(c0 // 512) % 3], dps[(c0 // 512) % 3]
                    self.mm(pb[:, :n], XTAb[:, dg, :], Ug[:, c0:c0 + n], True, True, [dt_, dUg], [dpb])
                    if d == 0:
                        self.act(Pd[:, c0:c0 + n], pb[:, :n], AF.Copy, [dpb], [dPd])
                    else:
                        lo, hi = c0, c0 + n
                        if lo < NC8:
                            m = min(hi, NC8) - lo
                            self.act(Pd[:, NL8 + lo:NL8 + lo + m], pb[:, 0:m], AF.Copy, [dpb], [dPd])
                        if hi > NC8:
                            a0 = max(lo, NC8)
                            self.act(Pd[:, a0 - NC8:hi - NC8], pb[:, a0 - lo:n], AF.Copy, [dpb], [dPd])
            for k in range(KK):
                sh = 1 << k
                if sh >= NJ:
                    break
                for d in range(2):
                    dg = d * 16 + g
                    Pd, dPd = P[d], dP[d]
                    R_, dR_ = Rm[ri % 4], dR[ri % 4]
                    ri += 1
                    self.I('dve', 'tensor_scalar', [dt_], [dR_], R_[:], idn[:], AKr[:, k, dg:dg + 1], None, ALU.mult)
                    self.I('dve', 'scalar_tensor_tensor', [dt_, dR_], [dR_], R_[:], cst[:, 0, :], AKi[:, k, dg:dg + 1], R_[:],
                           ALU.mult, ALU.add)
                    n_tot = NJ - sh
                    segs = [(c0, min(512, n_tot - c0)) for c0 in range(0, n_tot, 512)]
                    pend = []
                    for bi_, (c0, n) in enumerate(segs):
                        pb, dpb = ps[3 * d + bi_ % 3], dps[3 * d + bi_ % 3]
                        src0 = c0 if d == 0 else c0 + sh
                        self.mm(pb[:, :n], R_[:], Pd[:, src0:src0 + n], True, True, [dR_, dPd], [dpb])
                        pend.append((pb, dpb, c0, n))
                    for (pb, dpb, c0, n) in pend:
                        dst0 = c0 + sh if d == 0 else c0
                        self.I('dve', 'tensor_tensor', [dpb, dPd], [dPd], Pd[:, dst0:dst0 + n], Pd[:, dst0:dst0 + n], pb[:, :n],
                               ALU.add)
            for d in range(2):
                Pd, dPd = P[d], dP[d]
                Sd, dSd = Sx[d], dSx[d]
                if d == 0:
                    self.I('dve', 'memset', [], [dSd], Sd[:, 0:1], 0.0)
                    self.I('dve', 'tensor_copy', [dPd], [dSd], Sd[:, 1:NJ], Pd[:, 0:NJ - 1])
                else:
                    self.I('dve', 'tensor_copy', [dPd], [dSd], Sd[:, NC8:NJ], Pd[:, 1:NL8 + 1])
                    self.I('dve', 'tensor_copy', [dPd], [dSd], Sd[:, 0:NC8 - 1], Pd[:, NL8 + 1:NJ])
                    self.I('dve', 'memset', [], [dSd], Sd[:, NC8 - 1:NC8], 0.0)
            ygg, dygg = yg[g % 2], dyg[g % 2]
            for (c0, n) in blocks:
                pb, dpb = ps[6 + (c0 // 512) % 2], dps[6 + (c0 // 512) % 2]
                self.mm(pb[:, :n], KMb[:, g, :], Ug[:, c0:c0 + n], True, False, [dt_, dUg], [dpb])
                self.mm(pb[:, :n], YSb[:, g, :], Sx[0][:, c0:c0 + n], False, False, [dt_, dSx[0]], [dpb])
                self.mm(pb[:, :n], YSb[:, 16 + g, :], Sx[1][:, c0:c0 + n], False, True, [dt_, dSx[1]], [dpb])
                self.act(ygg[:, c0:c0 + n], pb[:, :n], AF.Copy, [dpb], [dygg])
            for t_ in range(8):
                self.dma('sp', self.y8[16 * g:16 * g + 16, t_, :], ygg[16 * t_:16 * t_ + 16, :], [dygg], [], [dd['y8']])
        gw = self.sb('d_gw', [128, 2, 256], BF16); gbv = tl('gbv', [128, 2]); dgw = Dep()
        self.dma('pool', gw[:], w['s5_glu_w'][l].rearrange('(kc p) n -> p kc n', p=128), [], [dgw])
        for c in range(2):
            self.dma('sp', gbv[:, c:c + 1], w['s5_glu_b'][l, c * 128:(c + 1) * 128].rearrange('(p o) -> p o', o=1), [], [dgw],
                     allow_slow_non_contiguous=True)
        CH = 512
        yv = [self.sb('d_yv%d' % i, [128, CH]) for i in range(2)]; dyv = [Dep(), Dep()]
        ga = self.sb('d_ga', [128, CH]); dga = Dep()
        gT = self.sb('d_gT', [128, 2, CH], BF16); gF = self.sb('d_gF', [128, 2, CH]); dgT = Dep()
        ob = [self.sb('d_ob%d' % i, [128, CH], BF16) for i in range(2)]; dob = [Dep(), Dep()]
        sg = self.sb('d_sg', [128, CH]); dsg = Dep()
        K2 = 2.0 * math.sqrt(2.0 / math.pi)
        oi = 0
        for j0 in range(0, NJ, 64):
            nj = min(64, NJ - j0)
            n = nj * 8
            for c in range(2):
                yy, dyy = yv[c], dyv[c]
                self.dma('sp', yy[:, :n].rearrange('p (s j) -> p s j', s=8), self.y8[c * 128:(c + 1) * 128, :, j0:j0 + nj],
                         [dd['y8']], [dyy])
                self.I('dve', 'tensor_tensor', [dyy], [dga], ga[:, :n], yy[:, :n], yy[:, :n], ALU.mult)
                self.I('dve', 'tensor_scalar', [dga], [dga], ga[:, :n], ga[:, :n], 0.044715, 1.0, ALU.mult, ALU.add)
                self.I('dve', 'tensor_tensor', [dga, dyy], [dga], ga[:, :n], ga[:, :n], yy[:, :n], ALU.mult)
                self.act(ga[:, :n], ga[:, :n], AF.Sigmoid, [dga], [dga], scale=K2)
                self.I('dve', 'tensor_tensor', [dga, dyy], [dgT], gF[:, c, :n], ga[:, :n], yy[:, :n], ALU.mult)
                self.I('dve', 'tensor_copy', [dgT], [dgT], gT[:, c, :n], gF[:, c, :n])
            for c in range(2):
                pb, dpb = ps[c], dps[c]
                for kc in range(2):
                    self.mm(pb[:, :n], gw[:, kc, c * 128:(c + 1) * 128], gT[:, kc, :n], kc == 0, kc == 1, [dgw, dgT], [dpb])
                self.act(sg[:, :n], pb[:, :n], AF.Sigmoid, [dpb, dgw], [dsg], bias=gbv[:, c:c + 1])
                o_, do_ = ob[oi % 2], dob[oi % 2]
                oi += 1
                self.I('dve', 'tensor_tensor', [dsg, dgT], [do_], o_[:, :n].rearrange('p (j s) -> p s j', s=8),
                       sg[:, :n].rearrange('p (s j) -> p s j', s=8), gF[:, c, :n].rearrange('p (s j) -> p s j', s=8), ALU.mult)
                self.dma('sp', self.catT[c * 128:(c + 1) * 128, 8 * j0:8 * j0 + n], o_[:, :n], [do_], [], [dd['catT']])


def make_inmap(inputs, b, NL):
    m = {}
    for k in inputs.keys() if hasattr(inputs, 'keys') else inputs.files:
        if k in ('x', 'c', 'ctx', 'c_ctx'):
            continue
        a = np.asarray(inputs[k])
        if k == 'ml_gate_b':
            a = a.reshape(a.shape[0], 16)
        m[k] = np.ascontiguousarray(a, dtype=np.float32)
    m['x'] = np.ascontiguousarray(np.asarray(inputs['x'])[b, :NL], dtype=np.float32)
    m['ctx'] = np.ascontiguousarray(np.asarray(inputs['ctx'])[b], dtype=np.float32)
    m['cvec'] = np.ascontiguousarray(np.stack([np.asarray(inputs['c'])[b], np.asarray(inputs['c_ctx'])]), dtype=np.float32)
    m.update(host_consts(NL))
    return m


def build_program(NL, debug=()):
    b = B(NL, debug=debug)
    b.setup()
    for l in range(DEPTH):
        b.stage_begin(); b.stage_mod(l)
        b.stage_begin(); b.stage_a_weights(l); b.stage_a(l)
        b.stage_begin(); b.stage_b(l)
        b.stage_begin(); b.stage_c(l)
        b.stage_begin(); b.stage_d(l)
        b.stage_begin(); b.stage_e(l, l == DEPTH - 1)
    b.finish(b.outs)
    return b


def kernel(**inputs):
    NL = inputs['x'].shape[1]
    nb = inputs['x'].shape[0]
    b = build_program(NL)
    in_maps = [make_inmap(inputs, i, NL) for i in range(nb)]
    res = run_bass_kernel_spmd(b.nc, in_maps, core_ids=list(range(nb)))
    return np.stack([np.asarray(r['out'], dtype=np.float32) for r in res.results], axis=0)
```

```python
import math
from contextlib import ExitStack

import numpy as np
import ml_dtypes
import concourse.bass as bass
import concourse.mybir as mybir
from concourse.bass_utils import run_bass_kernel_spmd

F32 = mybir.dt.float32
BF16 = mybir.dt.bfloat16
AF = mybir.ActivationFunctionType
ALU = mybir.AluOpType
AX = mybir.AxisListType

D = 1024
NCTX = 256
DEPTH = 2
N_IN = 1712
LN_EPS = 1e-5
ALPHA = (2 * DEPTH) ** 0.25
MLA_SCALE = 96 ** -0.5

ENGS = ('pe', 'dve', 'act', 'pool', 'sp')
SAME_SYNC = {'pe': False, 'dve': True, 'act': True, 'pool': True, 'sp': False}
DMA_RING = 8


class Dep:
    __slots__ = ('ws', 'r', 'rd', 'prevr')

    def __init__(self):
        self.ws = []
        self.r = {}
        self.rd = []
        self.prevr = []


class Ins:
    __slots__ = ('eng', 'fn', 'waits', 'signal', 'val', 'is_dma', 'didx')

    def __init__(self, eng, fn, is_dma=False):
        self.eng = eng
        self.fn = fn
        self.waits = []
        self.signal = False
        self.val = 0
        self.is_dma = is_dma
        self.didx = -1


class Sched:
    def __init__(self, nc, es):
        self.nc = nc
        self.lists = {e: [] for e in ENGS}
        self.sems = {e: es.enter_context(nc.semaphore('s_' + e)) for e in ENGS}
        self.rings = {q: [es.enter_context(nc.semaphore('d_%s%d' % (q, i))) for i in range(DMA_RING)]
                      for q in ('sp', 'pool', 'act')}
        self.dcount = {q: 0 for q in ('sp', 'pool', 'act')}
        self.dlist = {q: [] for q in ('sp', 'pool', 'act')}

    def _add(self, ins, R, W, Wm=()):
        ws = {}
        for d in R:
            for w in d.ws:
                ws[id(w)] = w
        for d in W:
            for w in d.ws:
                ws[id(w)] = w
            for r in d.r.values():
                ws[id(r)] = r
            for r in d.rd:
                ws[id(r)] = r
            for r in d.prevr:
                ws[id(r)] = r
        for d in Wm:
            if d.r or d.rd:
                d.prevr = list(d.r.values()) + list(d.rd) + list(d.ws)
                d.ws = []
                d.r = {}
                d.rd = []
            for r in d.prevr:
                ws[id(r)] = r
        for w in ws.values():
            if w is ins:
                continue
            if (not w.is_dma) and w.eng == ins.eng and not SAME_SYNC[ins.eng]:
                continue
            ins.waits.append(w)
        for d in R:
            if ins.is_dma:
                d.rd.append(ins)
            else:
                d.r[ins.eng] = ins
        for d in W:
            d.ws = [ins]
            d.r = {}
            d.rd = []
            d.prevr = []
        for d in Wm:
            d.ws.append(ins)
        self.lists[ins.eng].append(ins)
        return ins

    def op(self, eng, fn, R=(), W=(), Wm=()):
        return self._add(Ins(eng, fn), R, W, Wm)

    def dma(self, q, out, in_, R=(), W=(), Wm=(), **kw):
        ins = Ins(q, lambda e: e.dma_start(out=out, in_=in_, **kw), is_dma=True)
        ins.didx = self.dcount[q]
        self.dcount[q] += 1
        self.dlist[q].append(ins)
        return self._add(ins, R, W, Wm)

    def finalize(self):
        for e in ENGS:
            for ins in self.lists[e]:
                for w in ins.waits:
                    if not w.is_dma:
                        w.signal = True
        for e in ENGS:
            c = 0
            for ins in self.lists[e]:
                if ins.signal and not ins.is_dma:
                    c += 1
                    ins.val = c
        nc = self.nc

        def tok(w):
            if w.is_dma:
                return self.rings[w.eng][w.didx % DMA_RING], 16 * (w.didx // DMA_RING + 1)
            return self.sems[w.eng], w.val

        def run(ename, eng):
            seen = {}
            for ins in self.lists[ename]:
                waits = [tok(w) for w in ins.waits]
                if ins.is_dma and ins.didx >= DMA_RING:
                    waits.append(tok(self.dlist[ename][ins.didx - DMA_RING]))
                mx = {}
                for sem, val in waits:
                    k = id(sem)
                    if val > seen.get(k, 0) and val > mx.get(k, (None, 0))[1]:
                        mx[k] = (sem, val)
                for k, (sem, val) in mx.items():
                    eng.wait_ge(sem, val)
                    seen[k] = val
                if ins.fn is None:
                    continue
                r = ins.fn(eng)
                if ins.is_dma:
                    s, v = tok(ins)
                    r.then_inc(s, 16)
                elif ins.signal:
                    r.then_inc(self.sems[ename], 1)

        with nc.Block() as block:
            @block.tensor
            def _(e):
                run('pe', e)

            @block.vector
            def _(e):
                run('dve', e)

            @block.scalar
            def _(e):
                run('act', e)

            @block.gpsimd
            def _(e):
                run('pool', e)

            @block.sync
            def _(e):
                run('sp', e)


def _rope_tables(NL):
    T = NCTX + NL
    t = np.arange(NL)
    row = (t // 64).astype(np.float32)
    col = (t % 64).astype(np.float32)
    inv = (10000.0 ** (-np.arange(8, dtype=np.float32) / 8)).astype(np.float32)
    ar = row[None, :] * inv[:, None]
    ac = col[None, :] * inv[:, None]
    cos = np.ones((32, T), np.float32)
    sin = np.zeros((32, T), np.float32)
    cos[0:8, NCTX:] = np.cos(ar); cos[8:16, NCTX:] = np.cos(ar)
    cos[16:24, NCTX:] = np.cos(ac); cos[24:32, NCTX:] = np.cos(ac)
    sin[0:8, NCTX:] = np.sin(ar); sin[8:16, NCTX:] = np.sin(ar)
    sin[16:24, NCTX:] = np.sin(ac); sin[24:32, NCTX:] = np.sin(ac)
    return cos, sin


def host_consts(NL):
    cos, sin = _rope_tables(NL)
    ident = np.eye(128, dtype=np.float32)
    s = np.arange(128)
    tri_f = (s[:, None] <= s[None, :]).astype(np.float32)
    tri_b = (s[:, None] >= s[None, :]).astype(np.float32)
    sw = np.zeros((128, 128), np.float32)
    sw[np.arange(64), np.arange(64) + 64] = 1.0
    sw[np.arange(64) + 64, np.arange(64)] = 1.0
    blk = s // 16
    m8f = (blk[:, None] <= blk[None, :]).astype(np.float32)
    m8b = (blk[:, None] >= blk[None, :]).astype(np.float32)
    return {'k_cos': cos, 'k_sin': sin, 'k_ident': ident, 'k_trif': tri_f, 'k_trib': tri_b,
            'k_sw': sw, 'k_m8f': m8f, 'k_m8b': m8b}


class B:
    def __init__(self, NL, n_layers=DEPTH, debug=()):
        self.NL = NL
        self.T = NCTX + NL
        self.n_layers = n_layers
        self.debug = set(debug)
        self.nc = bass.Bass('TRN2', target_bir_lowering=False)
        self.es = ExitStack()
        self.S = Sched(self.nc, self.es)
        self.deps = {}
        self._n = 0
        self.outs = []

    def din(self, name, shape, dt=F32):
        return self.nc.dram_tensor(name, list(shape), dt, kind='ExternalInput').ap()

    def dscr(self, name, shape, dt=F32):
        kind = 'ExternalOutput' if name in self.debug else 'Internal'
        return self.nc.dram_tensor(name, list(shape), dt, kind=kind).ap()

    def sb(self, name, shape, dt=F32, persist=False):
        if not hasattr(self, 'arena'):
            self.arena = self.nc.alloc_sbuf_tensor('arena', [128, 51200], F32)
            self.a_lo = 0
            self.a_hi = 51200
        n = 1
        for d_ in shape[1:]:
            n *= d_
        words = n if dt == F32 else (n + 1) // 2
        words = (words + 7) // 8 * 8
        if persist:
            o = self.a_lo
            self.a_lo += words
        else:
            self.a_hi -= words
            o = self.a_hi
        assert self.a_lo <= self.a_hi, 'SBUF arena exhausted: %s' % name
        v = self.arena[:, o:o + (n if dt == F32 else (n + 1) // 2)]
        if dt != F32:
            v = v.bitcast(dt)
        if n % 2 and dt != F32:
            v = v[:, 0:n]
        P = shape[0]
        if len(shape) == 3:
            v = v.rearrange('p (a b) -> p a b', a=shape[1])
        elif len(shape) == 4:
            v = v.rearrange('p (a b c) -> p a b c', a=shape[1], b=shape[2])
        return v[0:P] if P < 128 else v

    def stage_begin(self):
        S = self.S
        lasts = []
        for e in ENGS:
            for ins in reversed(S.lists[e]):
                if ins.fn is not None and not ins.is_dma:
                    lasts.append(ins)
                    break
        for q in S.dlist:
            lasts.extend(S.dlist[q][-DMA_RING:])
        for e in ENGS:
            ins = Ins(e, None)
            ins.waits = [w for w in lasts if w.is_dma or w.eng != e]
            S.lists[e].append(ins)
        self.a_hi = 51200
        if hasattr(self, 'ln_st'):
            del self.ln_st

    def dep(self, name=None):
        d = Dep()
        return d

    def I(self, eng, method, R, W, *a, **kw):
        return self.S.op(eng, lambda e: getattr(e, method)(*a, **kw), R, W)

    def mm(self, out, lhsT, rhs, start, stop, R, W):
        return self.S.op('pe', lambda e: e.matmul(out, lhsT, rhs, start=start, stop=stop), R, W)

    def tp(self, out, in_, ident, R, W):
        return self.S.op('pe', lambda e: e.transpose(out, in_, ident), R, W)

    def act(self, out, in_, func, R, W, **kw):
        return self.S.op('act', lambda e: e.activation(out=out, in_=in_, func=func, **kw), R, W)

    def dma(self, q, out, in_, R, W, Wm=(), **kw):
        return self.S.dma(q, out, in_, R, W, Wm, **kw)

    def bank(self):
        i = self._n % 8
        self._n += 1
        return self.ps[i], self.dps[i]

    def setup(self):
        nc, NL, T = self.nc, self.NL, self.T
        L = DEPTH
        self.x = self.din('x', [NL, D])
        self.ctx = self.din('ctx', [NCTX, D])
        self.cvec = self.din('cvec', [2, D])
        shp = {
            'ada_w': [L, D, 6 * D], 'ada_b': [L, 6 * D], 'w_in': [L, D, N_IN],
            's5_lambda_re': [L, 2, 16, 64], 's5_lambda_im': [L, 2, 16, 64], 's5_log_step': [L, 2, 16],
            's5_b_re': [L, 2, 16, 64, 16], 's5_b_im': [L, 2, 16, 64, 16],
            's5_c_re': [L, 2, 16, 16, 64], 's5_c_im': [L, 2, 16, 16, 64], 's5_d': [L, 256],
            's5_glu_w': [L, 256, 256], 's5_glu_b': [L, 256], 'mla_q_norm': [L, 256],
            'mla_w_q_up': [L, 256, 768], 'mla_kv_norm': [L, 128], 'mla_w_kv_up': [L, 128, 1024],
            'ml_conv_w': [L, 3, 512], 'ml_conv_b': [L, 512], 'ml_gate_b': [L, 16], 'ml_norm_g': [L, 256],
            'w_out': [L, D, D], 'ln1_g': [L, D], 'ln1_b': [L, D], 'ln2_g': [L, D], 'ln2_b': [L, D],
            'router_w': [L, D, 64], 'router_bias': [L, 64],
            'exp_w_gate': [L, 64, D, 256], 'exp_w_up': [L, 64, D, 256], 'exp_w_down': [L, 64, 256, D],
            'sh_w_gate': [L, D, 256], 'sh_w_up': [L, D, 256], 'sh_w_down': [L, 256, D],
        }
        self.w = {k: self.din(k, v) for k, v in shp.items()}
        self.k = {'k_cos': self.din('k_cos', [32, T]), 'k_sin': self.din('k_sin', [32, T]),
                  'k_ident': self.din('k_ident', [128, 128]), 'k_trif': self.din('k_trif', [128, 128]),
                  'k_trib': self.din('k_trib', [128, 128]), 'k_sw': self.din('k_sw', [128, 128]),
                  'k_m8f': self.din('k_m8f', [128, 128]), 'k_m8b': self.din('k_m8b', [128, 128])}
        self.out = nc.dram_tensor('out', [NL, D], F32, kind='ExternalOutput').ap()
        self.xres = self.dscr('xres', [T, D])
        self.modv = self.dscr('modv', [2, 6 * D])
        self.zTu = self.dscr('zTu', [256, 8, T // 8], BF16)
        self.y8 = self.dscr('y8', [256, 8, T // 8])
        self.zTqk = self.dscr('zTqk', [512, T])
        self.QT = self.dscr('QT', [8, 96, T], BF16)
        self.KT = self.dscr('KT', [8, 96, T], BF16)
        self.Vv = self.dscr('Vv', [T, 512], BF16)
        self.mlV = self.dscr('mlV', [T, 256], BF16)
        self.mlO = self.dscr('mlO', [T, 256])
        self.mlG = self.dscr('mlG', [T, 16])
        self.catT = self.dscr('catT', [D, T], BF16)
        self.hfD = self.dscr('hfD', [T, 256])
        self.dd = {n: Dep() for n in ('xres', 'modv', 'zTu', 'zTqk', 'QT', 'KT', 'Vv', 'mlV', 'mlO', 'mlG',
                                      'catT', 'out', 'hfD', 'y8')}
        self.ps = [nc.alloc_psum_tensor('ps%d' % i, [128, 512], F32) for i in range(8)]
        self.dps = [Dep() for _ in range(8)]
        self.ident = self.sb('ident', [128, 128], persist=True); self.d_ident = Dep()
        self.identb = self.sb('identb', [128, 128], BF16, persist=True)
        self.dma('sp', self.ident[:], self.k['k_ident'], [], [self.d_ident])
        self.I('dve', 'tensor_copy', [self.d_ident], [self.d_ident], self.identb[:], self.ident[:])

    def finish(self, outs):
        ins = Ins('sp', None)
        ins.waits = list(outs)
        self.S.lists['sp'].append(ins)
        self.S.finalize()

    def stage_mod(self, l):
        nc = self.nc
        if not hasattr(self, 'modT'):
            self.modT = self.sb('modT', [128, 2, 6, 8], persist=True); self.d_modT = Dep()
            self.modbc = self.sb('modbc', [128, 2, 2, D], persist=True); self.d_modbc = Dep()
        self.cT = self.sb('cT', [128, 2, 8]); self.d_cT = Dep()
        self.adab = self.sb('adab', [2, 6 * D]); self.d_adab = Dep()
        self.modrow = self.sb('modrow', [2, 6 * D]); self.d_modrow = Dep()
        self.adaw = [self.sb('adaw%d' % i, [128, 8, 512]) for i in range(2)]
        self.d_adaw = [Dep(), Dep()]
        for r in range(2):
            self.dma('sp', self.cT[:, r, :], self.cvec[r].rearrange('(p kc) -> p kc', kc=8), [], [self.d_cT])
        self.act(self.cT[:], self.cT[:], AF.Silu, [self.d_cT], [self.d_cT])
        for r in range(2):
            self.dma('sp', self.adab[r:r + 1, :], self.w['ada_b'][l:l + 1, :], [], [self.d_adab])
        for nb in range(12):
            wt, dw = self.adaw[nb % 2], self.d_adaw[nb % 2]
            self.dma('sp', wt[:], self.w['ada_w'][l, :, nb * 512:(nb + 1) * 512].rearrange('(p kc) n -> p kc n', kc=8),
                     [], [dw])
            pb, dpb = self.bank()
            for kc in range(8):
                self.mm(pb[0:2, :], self.cT[:, :, kc], wt[:, kc, :], kc == 0, kc == 7, [self.d_cT, dw], [dpb])
            self.I('dve', 'tensor_tensor', [dpb, self.d_adab], [self.d_modrow],
                   self.modrow[:, nb * 512:(nb + 1) * 512], pb[0:2, :], self.adab[:, nb * 512:(nb + 1) * 512], ALU.add)
        o = self.dma('sp', self.modv, self.modrow[:], [self.d_modrow], [self.dd['modv']])
        for r in range(2):
            self.dma('sp', self.modT[:, r, :, :], self.modv[r].rearrange('(w kc p) -> p w kc', w=6, kc=8),
                     [self.dd['modv']], [self.d_modT], allow_slow_non_contiguous=True)
        for wq in (1, 4):
            self.I('dve', 'tensor_scalar', [self.d_modT], [self.d_modT], self.modT[:, :, wq, :], self.modT[:, :, wq, :],
                   1.0, None, ALU.add)
        for r in range(2):
            for gi, wq in enumerate((2, 5)):
                self.dma('sp', self.modbc[:, r, gi, :], self.modv[r:r + 1, wq * D:(wq + 1) * D].partition_broadcast(128),
                         [self.dd['modv']], [self.d_modbc])

    def ln_T(self, xt, dx, r, w_shift, w_scale, hT, dhT, c0, hT32=None):
        if not hasattr(self, 'ln_st'):
            self.ln_st = self.sb('ln_st', [128, 2, 6]); self.d_lnst = Dep()
            self.ln_mv = self.sb('ln_mv', [128, 2]); self.ln_rs = self.sb('ln_rs', [128, 2])
            self.d_lnrs = Dep()
            self.ln_xh = self.sb('ln_xh', [128, D]); self.d_lnxh = Dep()
        st, mv, rs, xh = self.ln_st, self.ln_mv, self.ln_rs, self.ln_xh
        for j in range(2):
            self.I('dve', 'bn_stats', [dx], [self.d_lnst], st[:, j, :], xt[:, j * 512:(j + 1) * 512])
        self.I('dve', 'bn_aggr', [self.d_lnst], [self.d_lnrs], mv[:], st[:].rearrange('p a b -> p (a b)'))
        self.act(rs[:, 0:1], mv[:, 1:2], AF.Sqrt, [self.d_lnrs], [self.d_lnrs], bias=LN_EPS)
        self.I('dve', 'reciprocal', [self.d_lnrs], [self.d_lnrs], rs[:, 0:1], rs[:, 0:1])
        self.I('dve', 'scalar_tensor_tensor', [self.d_lnrs], [self.d_lnrs], rs[:, 1:2], mv[:, 0:1], -1.0, rs[:, 0:1],
               ALU.mult, ALU.mult)
        self.act(xh[:], xt[:], AF.Identity, [dx, self.d_lnrs], [self.d_lnxh], scale=rs[:, 0:1], bias=rs[:, 1:2])
        for half in range(2):
            pb, dpb = self.bank()
            for q in range(4):
                kc = half * 4 + q
                self.tp(pb[:, q * 128:(q + 1) * 128], xh[:, kc * 128:(kc + 1) * 128], self.ident[:],
                        [self.d_lnxh, self.d_ident], [dpb])
            for q in range(4):
                kc = half * 4 + q
                self.I('dve', 'tensor_scalar', [dpb, self.d_modT], [dhT], hT[:, kc, c0:c0 + 128],
                       pb[:, q * 128:(q + 1) * 128], self.modT[:, r, w_scale, kc:kc + 1],
                       self.modT[:, r, w_shift, kc:kc + 1], ALU.mult, ALU.add)
                if hT32 is not None:
                    self.act(hT32[:, kc, 0:128], pb[:, q * 128:(q + 1) * 128], AF.Identity,
                             [dpb, self.d_modT], [dhT], scale=self.modT[:, r, w_scale, kc:kc + 1],
                             bias=self.modT[:, r, w_shift, kc:kc + 1])

    def macro_tiles(self):
        out = [(0, 2, 1)]
        t = NCTX
        while t < self.T:
            out.append((t, 4, 0))
            t += 512
        return out

    def src_rows(self, l, t0):
        if l == 0:
            if t0 < NCTX:
                return self.ctx[t0:t0 + 128, :]
            return self.x[t0 - NCTX:t0 - NCTX + 128, :]
        return self.xres[t0:t0 + 128, :]

    def stage_a_weights(self, l):
        if True:
            self.win = self.sb('win', [128, 8, N_IN], BF16); self.d_win = Dep()
            self.wkr = self.sb('wkr', [128, 8, 2, 96], BF16)
            self.wq = self.sb('wq', [128, 2, 768], BF16); self.d_wq = Dep()
            self.wqs = self.sb('wqs', [128, 2, 768], BF16)
            self.wkv = self.sb('wkv', [128, 1024], BF16); self.d_wkv = Dep()
            self.wkvV = self.sb('wkvV', [128, 512], BF16)
            self.qg = self.sb('qg', [128, 256]); self.kvg = self.sb('kvg', [128, 128]); self.d_g = Dep()
            self.gb = self.sb('gb', [128, 16])
        w = self.w
        self.dma('pool', self.win[:], w['w_in'][l].rearrange('(kc p) n -> p kc n', p=128), [], [self.d_win])
        self.dma('pool', self.wq[:], w['mla_w_q_up'][l].rearrange('(kc p) n -> p kc n', p=128), [], [self.d_wq])
        self.dma('pool', self.wkv[:], w['mla_w_kv_up'][l], [], [self.d_wkv])
        self.dma('sp', self.qg[:], w['mla_q_norm'][l:l + 1, :].partition_broadcast(128), [], [self.d_g])
        self.dma('sp', self.kvg[:], w['mla_kv_norm'][l:l + 1, :].partition_broadcast(128), [], [self.d_g])
        self.dma('sp', self.gb[:], w['ml_gate_b'][l:l + 1, :].partition_broadcast(128), [], [self.d_g])
        self.I('pool', 'memset', [], [self.d_win], self.wkr[:], 0.0)
        self.I('dve', 'memset', [], [self.d_wq], self.wqs[:], 0.0)
        self.I('dve', 'tensor_copy', [self.d_win], [self.d_win], self.wkr[:, :, 0, 64:96], self.win[:, :, 640:672])
        for a in range(2):
            o = 16 * a
            self.I('dve', 'tensor_scalar', [self.d_win], [self.d_win], self.wkr[:, :, 1, 64 + o:72 + o],
                   self.win[:, :, 648 + o:656 + o], -1.0, None, ALU.mult)
            self.I('dve', 'tensor_copy', [self.d_win], [self.d_win], self.wkr[:, :, 1, 72 + o:80 + o],
                   self.win[:, :, 640 + o:648 + o])
        for h in range(8):
            for a in range(2):
                o = h * 96 + 64 + 16 * a
                self.I('dve', 'tensor_scalar', [self.d_wq], [self.d_wq], self.wqs[:, :, o:o + 8],
                       self.wq[:, :, o + 8:o + 16], -1.0, None, ALU.mult)
                self.I('dve', 'tensor_copy', [self.d_wq], [self.d_wq], self.wqs[:, :, o + 8:o + 16],
                       self.wq[:, :, o:o + 8])
            self.I('dve', 'tensor_copy', [self.d_wkv], [self.d_wkv], self.wkvV[:, h * 64:(h + 1) * 64],
                   self.wkv[:, h * 128 + 64:h * 128 + 128])

    def stage_a(self, l):
        nc = self.nc
        if True:
            self.a_x = [self.sb('a_x%d' % i, [128, D]) for i in range(2)]; self.d_ax = [Dep(), Dep()]
            self.a_hT = self.sb('a_hT', [128, 8, 512], BF16); self.d_ahT = Dep()
            self.a_fm = [self.sb('a_fm%d' % i, [128, 512], BF16) for i in range(2)]; self.d_afm = [Dep(), Dep()]
            self.a_fm32 = [self.sb('a_fm32_%d' % i, [128, 512]) for i in range(2)]; self.d_afm32 = [Dep(), Dep()]
            self.a_cs = self.sb('a_cs', [96, 2, 512]); self.d_acs = Dep()
            self.a_rt = self.sb('a_rt', [96, 2, 512]); self.d_art = Dep()
            self.a_kr = self.sb('a_kr', [96, 512], BF16); self.d_akr = Dep()
            self.a_sq = self.sb('a_sq', [128, 256]); self.a_ss = self.sb('a_ss', [128, 4]); self.d_ass = Dep()
            self.a_qn = self.sb('a_qn', [128, 384], BF16); self.d_aqn = Dep()
            self.a_qnT = self.sb('a_qnT', [128, 3, 512], BF16); self.d_aqnT = Dep()
            self.a_gt = self.sb('a_gt', [128, 16]); self.a_ge = self.sb('a_ge', [128, 8]); self.d_agt = Dep()
            self.a_v = self.sb('a_v', [128, 256], BF16); self.d_av = Dep()
            self.a_o = self.sb('a_o', [128, 256]); self.d_ao = Dep()
            self.a_q = [self.sb('a_q%d' % i, [96, 512], BF16) for i in range(2)]; self.d_aq = [Dep(), Dep()]
            self.a_vv = self.sb('a_vv', [128, 512], BF16); self.d_avv = Dep()
        dd = self.dd
        win, dwin = self.win, self.d_win
        cnt = 0
        for (t0, nsub, r) in self.macro_tiles():
            N = nsub * 128
            hT, dhT = self.a_hT, self.d_ahT
            self.dma('sp', self.a_cs[64:96, 0, :N], self.k['k_cos'][:, t0:t0 + N], [], [self.d_acs])
            self.dma('sp', self.a_cs[64:96, 1, :N], self.k['k_sin'][:, t0:t0 + N], [], [self.d_acs])
            for s in range(nsub):
                xt, dx = self.a_x[s % 2], self.d_ax[s % 2]
                self.dma('sp', xt[:], self.src_rows(l, t0 + s * 128), [dd['xres']], [dx])
                self.ln_T(xt, dx, r, 0, 1, hT, dhT, s * 128)
            fm = [(0, 'u', 0), (128, 'u', 128), (672, 'qk', 0), (800, 'qk', 128), (928, 'qk', 256), (1056, 'qk', 384)]
            for (c0, kind, ro) in fm:
                pb, dpb = self.bank()
                for kc in range(8):
                    self.mm(pb[:, :N], win[:, kc, c0:c0 + 128], hT[:, kc, :N], kc == 0, kc == 7, [dwin, dhT], [dpb])
                cnt += 1
                if kind == 'u':
                    ft, dft = self.a_fm[cnt % 2], self.d_afm[cnt % 2]
                    self.I('dve', 'tensor_copy', [dpb], [dft], ft[:, :N].rearrange('p (s j) -> p s j', s=8),
                           pb[:, :N].rearrange('p (j s) -> p s j', s=8))
                    self.dma('sp', self.zTu[ro:ro + 128, :, t0 // 8:(t0 + N) // 8], ft[:, :N].rearrange('p (s j) -> p s j', s=8),
                             [dft], [], [dd['zTu']])
                else:
                    ft, dft = self.a_fm32[cnt % 2], self.d_afm32[cnt % 2]
                    self.act(ft[:, :N], pb[:, :N], AF.Copy, [dpb], [dft])
                    self.dma('sp', self.zTqk[ro:ro + 128, t0:t0 + N], ft[:, :N], [dft], [], [dd['zTqk']])
            pbs = []
            for v in range(2):
                pb, dpb = self.bank()
                for kc in range(8):
                    self.mm(pb[0:96, :N], self.wkr[:, kc, v, :], hT[:, kc, :N], kc == 0, kc == 7, [dwin, dhT], [dpb])
                pbs.append((pb, dpb))
            self.rope(pbs, N, self.a_kr, self.d_akr)
            for h in range(8):
                self.dma('sp', self.KT[h, 64:96, t0:t0 + N], self.a_kr[64:96, :N], [self.d_akr], [], [dd['KT']])
            for s in range(nsub):
                tt = t0 + s * 128
                cs = slice(s * 128, (s + 1) * 128)
                pa, dpa = self.bank()
                for kc in range(8):
                    self.mm(pa[:, 0:384], hT[:, kc, cs], win[:, kc, 256:640], kc == 0, kc == 7, [dwin, dhT], [dpa])
                for kc in range(8):
                    self.mm(pa[:, 384:400], hT[:, kc, cs], win[:, kc, 1696:1712], kc == 0, kc == 7, [dwin, dhT], [dpa])
                pbk, dpbk = self.bank()
                for kc in range(8):
                    self.mm(pbk[:, :], hT[:, kc, cs], win[:, kc, 1184:1696], kc == 0, kc == 7, [dwin, dhT], [dpbk])
                ss = self.a_ss
                self.act(self.a_sq[:, 0:256], pa[:, 0:256], AF.Square, [dpa], [self.d_ass], accum_out=ss[:, 0:1])
                self.act(self.a_sq[:, 0:128], pa[:, 256:384], AF.Square, [dpa, self.d_ass], [self.d_ass],
                         accum_out=ss[:, 1:2])
                self.act(ss[:, 2:3], ss[:, 0:1], AF.Sqrt, [self.d_ass], [self.d_ass], scale=1.0 / 256, bias=1e-6)
                self.act(ss[:, 3:4], ss[:, 1:2], AF.Sqrt, [self.d_ass], [self.d_ass], scale=1.0 / 128, bias=1e-6)
                self.I('dve', 'reciprocal', [self.d_ass], [self.d_ass], ss[:, 2:4], ss[:, 2:4])
                self.I('dve', 'scalar_tensor_tensor', [dpa, self.d_ass, self.d_g], [self.d_aqn], self.a_qn[:, 0:256],
                       pa[:, 0:256], ss[:, 2:3], self.qg[:], ALU.mult, ALU.mult)
                self.I('dve', 'scalar_tensor_tensor', [dpa, self.d_ass, self.d_g], [self.d_aqn], self.a_qn[:, 256:384],
                       pa[:, 256:384], ss[:, 3:4], self.kvg[:], ALU.mult, ALU.mult)
                pt, dpt = self.bank()
                ptb = pt[:].bitcast(BF16)
                for j in range(3):
                    self.tp(ptb[:, j * 128:(j + 1) * 128], self.a_qn[:, j * 128:(j + 1) * 128], self.identb[:],
                            [self.d_aqn, self.d_ident], [dpt])
                for j in range(3):
                    self.I('dve', 'tensor_copy', [dpt], [self.d_aqnT], self.a_qnT[:, j, cs], ptb[:, j * 128:(j + 1) * 128])
                gt, ge = self.a_gt, self.a_ge
                self.I('dve', 'tensor_tensor', [dpa, self.d_g], [self.d_agt], gt[:], pa[:, 384:400], self.gb[:], ALU.add)
                self.act(ge[:], gt[:, 8:16], AF.Exp, [self.d_agt], [self.d_agt], scale=-1.0)
                self.act(ge[:], ge[:], AF.Ln, [self.d_agt], [self.d_agt], bias=1.0)
                self.I('dve', 'tensor_scalar', [self.d_agt], [self.d_agt], gt[:, 8:16], ge[:], -1.0, None, ALU.mult)
                self.dma('sp', self.mlG[tt:tt + 128, :], gt[:], [self.d_agt], [], [dd['mlG']])
                self.I('dve', 'tensor_copy', [dpbk], [self.d_av], self.a_v[:], pbk[:, 0:256])
                self.dma('sp', self.mlV[tt:tt + 128, :], self.a_v[:], [self.d_av], [], [dd['mlV']])
                self.act(self.a_o[:], pbk[:, 256:512], AF.Sigmoid, [dpbk], [self.d_ao])
                self.dma('sp', self.mlO[tt:tt + 128, :], self.a_o[:], [self.d_ao], [], [dd['mlO']])
            qnT, dq = self.a_qnT, self.d_aqnT
            for h in range(8):
                pbs = []
                for v, wt in enumerate((self.wq, self.wqs)):
                    pb, dpb = self.bank()
                    for j in range(2):
                        self.mm(pb[0:96, :N], wt[:, j, h * 96:(h + 1) * 96], qnT[:, j, :N], j == 0, j == 1,
                                [self.d_wq, dq], [dpb])
                    pbs.append((pb, dpb))
                qt, dqt = self.a_q[h % 2], self.d_aq[h % 2]
                self.rope(pbs, N, qt, dqt)
                self.I('dve', 'tensor_copy', [pbs[0][1]], [dqt], qt[0:64, :N], pbs[0][0][0:64, :N])
                self.dma('sp', self.QT[h, :, t0:t0 + N], qt[:, :N], [dqt], [], [dd['QT']])
                pb, dpb = self.bank()
                self.mm(pb[0:64, :N], self.wkv[:, h * 128:h * 128 + 64], qnT[:, 2, :N], True, True, [self.d_wkv, dq], [dpb])
                kt, dkt = self.a_fm[h % 2], self.d_afm[h % 2]
                self.act(kt[0:64, :N], pb[0:64, :N], AF.Copy, [dpb], [dkt])
                self.dma('sp', self.KT[h, 0:64, t0:t0 + N], kt[0:64, :N], [dkt], [], [dd['KT']])
            for s in range(nsub):
                tt = t0 + s * 128
                pb, dpb = self.bank()
                self.mm(pb[:, :], qnT[:, 2, s * 128:(s + 1) * 128], self.wkvV[:], True, True, [dq, self.d_wkv], [dpb])
                self.I('dve', 'tensor_copy', [dpb], [self.d_avv], self.a_vv[:], pb[:, :])
                self.dma('sp', self.Vv[tt:tt + 128, :], self.a_vv[:], [self.d_avv], [], [dd['Vv']])

    def rope(self, pbs, N, out, dout):
        cs, rt = self.a_cs, self.a_rt
        (p0, d0), (p1, d1) = pbs
        self.I('dve', 'tensor_tensor', [d0, self.d_acs], [self.d_art], rt[64:96, 0, :N], p0[64:96, :N], cs[64:96, 0, :N],
               ALU.mult)
        self.I('dve', 'tensor_tensor', [d1, self.d_acs], [self.d_art], rt[64:96, 1, :N], p1[64:96, :N], cs[64:96, 1, :N],
               ALU.mult)
        self.I('dve', 'tensor_tensor', [self.d_art], [dout], out[64:96, :N], rt[64:96, 0, :N], rt[64:96, 1, :N], ALU.add)


    def stage_b(self, l):
        T, NL = self.T, self.NL
        nkc = T // 128
        if True:
            self.b_K = [self.sb('b_K%d' % i, [96, T], BF16) for i in range(2)]; self.d_bK = [Dep(), Dep()]
            self.b_V = [self.sb('b_V%d' % i, [128, nkc, 65], BF16) for i in range(2)]; self.d_bV = [Dep(), Dep()]
            self.b_Q = [self.sb('b_Q%d' % i, [96, 512], BF16) for i in range(2)]; self.d_bQ = [Dep(), Dep()]
            self.b_P = [self.sb('b_P%d' % i, [128, 512], BF16) for i in range(4)]; self.d_bP = [Dep() for _ in range(4)]
            self.b_E = self.sb('b_E', [65, 64]); self.d_bE = Dep()
            self.b_o = self.sb('b_o', [65, 512]); self.d_bo = Dep()
            self.b_rec = self.sb('b_rec', [64, 512]); self.d_brec = Dep()
            self.b_ob = [self.sb('b_ob%d' % i, [64, 512], BF16) for i in range(2)]; self.d_bob = [Dep(), Dep()]
            self.I('dve', 'memset', [], [self.d_bE], self.b_E[:], 0.0)
            self.I('dve', 'memset', [], [self.d_bE], self.b_E[64:65, :], 1.0)
            for i in range(2):
                self.I('dve', 'memset', [], [self.d_bV[i]], self.b_V[i][:, :, 64:65], 1.0)
        dd = self.dd
        ps, dps = self.ps, self.dps
        blocks = [(0, NCTX, 2)]
        t = NCTX
        while t < T:
            blocks.append((t, 512, nkc))
            t += 512
        si = 0
        bi = 0
        LA = 2
        for h in range(8):
            K, dK = self.b_K[h % 2], self.d_bK[h % 2]
            V, dV = self.b_V[h % 2], self.d_bV[h % 2]
            self.dma('sp', K[:], self.KT[h], [dd['KT']], [dK])
            self.dma('sp', V[:, :, 0:64], self.Vv[:, h * 64:(h + 1) * 64].rearrange('(kc p) c -> p kc c', p=128),
                     [dd['Vv']], [dV])
            items = []
            for (t0, N, nk) in blocks:
                for kc in range(nk):
                    items.append((bi, t0, N, kc, nk))
                bi += 1
            pend = {}
            for step in range(len(items) + LA):
                if step < len(items):
                    (b_, t0, N, kc, nk) = items[step]
                    Q, dQ = self.b_Q[b_ % 2], self.d_bQ[b_ % 2]
                    if kc == 0:
                        self.dma('sp', Q[:, :N], self.QT[h, :, t0:t0 + N], [dd['QT']], [dQ])
                    pb, dpb = ps[si % 4], dps[si % 4]
                    P, dP = self.b_P[si % 4], self.d_bP[si % 4]
                    si += 1
                    self.mm(pb[:, :N], K[:, kc * 128:(kc + 1) * 128], Q[:, :N], True, True, [dK, dQ], [dpb])
                    self.act(P[:, :N], pb[:, :N], AF.Exp, [dpb], [dP], scale=MLA_SCALE)
                    pend[step] = (P, dP)
                j = step - LA
                if j >= 0:
                    (b_, t0, N, kc, nk) = items[j]
                    P, dP = pend.pop(j)
                    po, dpo = ps[4 + b_ % 2], dps[4 + b_ % 2]
                    self.mm(po[0:65, :N], V[:, kc, :], P[:, :N], kc == 0, kc == nk - 1, [dV, dP], [dpo])
                    if kc == nk - 1:
                        self.I('dve', 'tensor_copy', [dpo], [self.d_bo], self.b_o[:, :N], po[0:65, :N])
                        pd, dpd = ps[6], dps[6]
                        self.mm(pd[0:64, :N], self.b_E[:], self.b_o[:, :N], True, True, [self.d_bE, self.d_bo], [dpd])
                        self.I('dve', 'reciprocal', [dpd], [self.d_brec], self.b_rec[:, :N], pd[0:64, :N])
                        ob, dob = self.b_ob[b_ % 2], self.d_bob[b_ % 2]
                        self.I('dve', 'tensor_tensor', [self.d_bo, self.d_brec], [dob], ob[:, :N], self.b_o[0:64, :N],
                               self.b_rec[:, :N], ALU.mult)
                        self.dma('sp', self.catT[256 + h * 64:256 + (h + 1) * 64, t0:t0 + N], ob[:, :N], [dob], [],
                                 [dd['catT']])

    def stage_c(self, l):
        T, NL = self.T, self.NL
        nch = T // 128
        dd = self.dd
        if True:
            self.c_x = self.sb('c_x', [128, T]); self.d_cx = Dep()
            self.c_acc = self.sb('c_acc', [128, T]); self.d_cacc = Dep()
            self.c_qk = self.sb('c_qk', [128, 4, T], BF16); self.d_cqk = Dep()
            self.c_cw = self.sb('c_cw', [128, 4, 4]); self.d_ccw = Dep()
            self.c_tri = self.sb('c_tri', [128, 2, 128]); self.d_ctri = Dep()
            self.c_trib = self.sb('c_trib', [128, 2, 128], BF16)
            self.c_ones = self.sb('c_ones', [128, 128])
            self.c_G = self.sb('c_G', [128, 16]); self.d_cG = Dep()
            self.c_gs = self.sb('c_gs', [128, 16]); self.d_cgs = Dep()
            self.c_al = self.sb('c_al', [128, 4]); self.d_cal = Dep()
            self.c_V = [self.sb('c_V%d' % i, [128, 4, 65], BF16) for i in range(2)]; self.d_cV = [Dep(), Dep()]
            self.c_P = [self.sb('c_P%d' % i, [128, 128], BF16) for i in range(2)]; self.d_cP = [Dep(), Dep()]
            self.c_kb = [self.sb('c_kb%d' % i, [128, 64], BF16) for i in range(2)]; self.d_ckb = [Dep(), Dep()]
            self.c_C32 = self.sb('c_C32', [128, 4, 65]); self.c_Cb = self.sb('c_Cb', [128, 4, 65], BF16)
            self.d_cC = [Dep() for _ in range(4)]
            self.c_La = self.sb('c_La', [128, 65]); self.d_cLa = Dep()
            self.c_hfw = [self.sb('c_hfw%d' % i, [128, 256]) for i in range(2)]; self.d_chfw = [Dep(), Dep()]
            self.c_hfr = self.sb('c_hfr', [128, 256]); self.d_chfr = Dep()
            self.c_h = self.sb('c_h', [128, 256]); self.d_ch = Dep()
            self.c_w = self.sb('c_w', [128, 8]); self.d_cw = Dep()
            self.c_O = self.sb('c_O', [128, 256]); self.d_cO = Dep()
            self.c_ng = self.sb('c_ng', [128, 256]); self.d_cng = Dep()
            self.c_st = self.sb('c_st', [128, 4, 6]); self.c_mv = self.sb('c_mv', [128, 4, 2]); self.d_cst = Dep()
            self.c_rs = self.sb('c_rs', [128, 4])
            self.c_hn = self.sb('c_hn', [128, 256], BF16); self.d_chn = Dep()
            self.c_hT = self.sb('c_hT', [128, 2, 128], BF16); self.d_chT = Dep()
            self.dma('sp', self.c_tri[:, 0, :], self.k['k_trif'], [], [self.d_ctri])
            self.dma('sp', self.c_tri[:, 1, :], self.k['k_trib'], [], [self.d_ctri])
            self.I('dve', 'tensor_copy', [self.d_ctri], [self.d_ctri], self.c_trib[:], self.c_tri[:])
            self.I('dve', 'memset', [], [self.d_ctri], self.c_ones[:], 1.0)
            for i in range(2):
                self.I('dve', 'memset', [], [self.d_cV[i]], self.c_V[i][:, :, 64:65], 1.0)
        w = self.w
        for c in range(4):
            for j in range(3):
                self.dma('sp', self.c_cw[:, c, j:j + 1], w['ml_conv_w'][l, j, c * 128:(c + 1) * 128].rearrange('(p o) -> p o', o=1),
                         [], [self.d_ccw], allow_slow_non_contiguous=True)
            self.dma('sp', self.c_cw[:, c, 3:4], w['ml_conv_b'][l, c * 128:(c + 1) * 128].rearrange('(p o) -> p o', o=1),
                     [], [self.d_ccw], allow_slow_non_contiguous=True)
        self.dma('sp', self.c_ng[:], w['ml_norm_g'][l:l + 1, :].partition_broadcast(128), [], [self.d_cng])
        x, acc, cw = self.c_x, self.c_acc, self.c_cw
        for c in range(4):
            self.dma('sp', x[:], self.zTqk[c * 128:(c + 1) * 128, :], [dd['zTqk']], [self.d_cx])
            self.I('dve', 'tensor_scalar', [self.d_cx, self.d_ccw], [self.d_cacc], acc[:], x[:], cw[:, c, 1:2], cw[:, c, 3:4],
                   ALU.mult, ALU.add)
            for (lo, hi) in ((1, NCTX), (NCTX + 1, T)):
                self.I('dve', 'scalar_tensor_tensor', [self.d_cx, self.d_ccw, self.d_cacc], [self.d_cacc], acc[:, lo:hi],
                       x[:, lo - 1:hi - 1], cw[:, c, 0:1], acc[:, lo:hi], ALU.mult, ALU.add)
            for (lo, hi) in ((0, NCTX - 1), (NCTX, T - 1)):
                self.I('dve', 'scalar_tensor_tensor', [self.d_cx, self.d_ccw, self.d_cacc], [self.d_cacc], acc[:, lo:hi],
                       x[:, lo + 1:hi + 1], cw[:, c, 2:3], acc[:, lo:hi], ALU.mult, ALU.add)
            self.act(acc[:], acc[:], AF.Silu, [self.d_cacc], [self.d_cacc])
            self.I('dve', 'tensor_scalar', [self.d_cacc], [self.d_cqk], self.c_qk[:, c, :], acc[:],
                   0.125 if c >= 2 else 1.0, None, ALU.mult)
        qk, dqk = self.c_qk, self.d_cqk
        orders = [list(range(nch)), [1, 0] + list(range(nch - 1, 1, -1))]
        Gt = [self.sb('c_Gt%d' % i, [128, 16]) for i in range(2)]; dGt = [Dep(), Dep()]
        gsb = [self.sb('c_gsb%d' % i, [128, 16]) for i in range(2)]; dgsb = [Dep(), Dep()]
        alb = [self.sb('c_alb%d' % i, [128, 4]) for i in range(2)]; dalb = [Dep(), Dep()]
        Vb = [self.sb('c_Vb%d' % i, [128, 4, 65], BF16) for i in range(3)]; dVb = [Dep(), Dep(), Dep()]
        Pb = [self.sb('c_Pb%d' % i, [128, 4, 128], BF16) for i in range(2)]; dPb = [Dep(), Dep()]
        Lab = [self.sb('c_Lab%d' % i, [128, 4, 65]) for i in range(2)]; dLab = [Dep(), Dep()]
        for i in range(3):
            self.I('dve', 'memset', [], [dVb[i]], Vb[i][:, :, 64:65], 1.0)
        ps, dps = self.ps, self.dps
        cnt = {'l': 0, 'k': 0}

        def local(d, j):
            li_ = cnt['l']
            cnt['l'] += 1
            cs = slice(j * 128, (j + 1) * 128)
            G, dG = Gt[li_ % 2], dGt[li_ % 2]
            gs, dgs = gsb[li_ % 2], dgsb[li_ % 2]
            al, dal = alb[li_ % 2], dalb[li_ % 2]
            V, dV = Vb[li_ % 3], dVb[li_ % 3]
            P, dP = Pb[li_ % 2], dPb[li_ % 2]
            La, dLa = Lab[li_ % 2], dLab[li_ % 2]
            self.dma('sp', G[:], self.mlG[cs, :], [dd['mlG']], [dG])
            self.dma('sp', V[:, :, 0:64], self.mlV[cs, :].rearrange('p (h c) -> p h c', h=4), [dd['mlV']], [dV])
            pg, dpg = ps[0], dps[0]
            self.mm(pg[:, 0:4], self.c_tri[:, d, :], G[:, 8 + 4 * d:12 + 4 * d], True, True, [self.d_ctri, dG], [dpg])
            self.mm(pg[:, 8:12], self.c_ones[:], G[:, 8 + 4 * d:12 + 4 * d], True, True, [self.d_ctri, dG], [dpg])
            self.I('dve', 'tensor_copy', [dpg], [dgs], gs[:, 0:4], pg[:, 0:4])
            self.I('dve', 'tensor_tensor', [dG, dgs], [dgs], gs[:, 4:8], G[:, 4 * d:4 * d + 4], gs[:, 0:4], ALU.subtract)
            self.act(gs[:, 4:8], gs[:, 4:8], AF.Exp, [dgs], [dgs])
            self.act(gs[:, 8:12], gs[:, 0:4], AF.Exp, [dgs], [dgs])
            self.act(al[:], pg[:, 8:12], AF.Exp, [dpg], [dal])
            pk, dpk = ps[1], dps[1]
            pkb = pk[:].bitcast(BF16)
            for c2 in range(2):
                self.tp(pkb[:, c2 * 128:(c2 + 1) * 128], qk[:, 2 + c2, cs], self.identb[:], [dqk, self.d_ident], [dpk])
            for h in range(4):
                c2, r0 = h // 2, (h % 2) * 64
                rr = slice(r0, r0 + 64)
                kT = qk[rr, 2 + c2, cs]
                qT = qk[rr, c2, cs]
                k_ = cnt['k']
                cnt['k'] += 1
                pS, dpS = ps[4 + k_ % 2], dps[4 + k_ % 2]
                pL, dpL = ps[6 + k_ % 2], dps[6 + k_ % 2]
                kb, dkb = self.c_kb[k_ % 2], self.d_ckb[k_ % 2]
                self.mm(pS[:, 0:128], kT, qT, True, True, [dqk], [dpS])
                self.I('dve', 'scalar_tensor_tensor', [dpS, dgs, self.d_ctri], [dP], P[:, h, :], pS[:, 0:128],
                       gs[:, 4 + h:5 + h], self.c_trib[:, d, :], ALU.mult, ALU.mult)
                self.I('dve', 'tensor_scalar', [dpk, dgs], [dkb], kb[:], pkb[:, h * 64:(h + 1) * 64],
                       gs[:, 4 + h:5 + h], None, ALU.mult)
                self.mm(pL[rr, 0:65], kb[:], V[:, h, :], True, True, [dkb, dV], [dpL])
                self.act(La[rr, h, :], pL[rr, 0:65], AF.Copy, [dpL, dal], [dLa], scale=al[rr, h:h + 1])
            return (d, j, gs, dgs, al, dal, V, dV, P, dP, La, dLa)

        def chain(loc, vi):
            (d, j, gs, dgs, al, dal, V, dV, P, dP, La, dLa) = loc
            cs = slice(j * 128, (j + 1) * 128)
            po, dpo = ps[2 + vi % 2], dps[2 + vi % 2]
            for h in range(4):
                c2, r0 = h // 2, (h % 2) * 64
                rr = slice(r0, r0 + 64)
                qT = qk[rr, c2, cs]
                self.mm(po[:, h * 65:(h + 1) * 65], P[:, h, :], V[:, h, :], True, False, [dP, dV], [dpo])
                self.mm(po[:, h * 65:(h + 1) * 65], qT, self.c_Cb[rr, h, :], False, True, [dqk, self.d_cC[h]], [dpo])
                self.I('dve', 'scalar_tensor_tensor', [dLa, dal, self.d_cC[h]], [self.d_cC[h]],
                       self.c_C32[rr, h, :], self.c_C32[rr, h, :], al[rr, h:h + 1], La[rr, h, :], ALU.mult, ALU.add)
                self.I('dve', 'tensor_copy', [self.d_cC[h]], [self.d_cC[h]], self.c_Cb[rr, h, :], self.c_C32[rr, h, :])
            wv = self.c_w
            pov = po[:, 0:260].rearrange('p (h c) -> p h c', h=4)
            self.I('dve', 'tensor_tensor', [dpo, dgs], [self.d_cw], wv[:, 0:4], pov[:, :, 64], gs[:, 8:12], ALU.mult)
            self.act(wv[:, 0:4], wv[:, 0:4], AF.Abs, [self.d_cw], [self.d_cw])
            self.I('dve', 'tensor_scalar', [self.d_cw], [self.d_cw], wv[:, 0:4], wv[:, 0:4], 1.0, None, ALU.max)
            self.I('dve', 'reciprocal', [self.d_cw], [self.d_cw], wv[:, 0:4], wv[:, 0:4])
            self.I('dve', 'tensor_tensor', [self.d_cw, dgs], [self.d_cw], wv[:, 4:8], wv[:, 0:4], gs[:, 8:12], ALU.mult)
            if d == 0:
                hw_, dhw = self.c_hfw[vi % 2], self.d_chfw[vi % 2]
                for h in range(4):
                    self.I('dve', 'tensor_scalar', [dpo, self.d_cw], [dhw], hw_[:, h * 64:(h + 1) * 64],
                           pov[:, h, 0:64], wv[:, 4 + h:5 + h], None, ALU.mult)
                self.dma('sp', self.hfD[cs, :], hw_[:], [dhw], [], [dd['hfD']])
            else:
                hh = self.c_h
                self.dma('sp', self.c_O[:], self.mlO[cs, :], [dd['mlO']], [self.d_cO])
                self.dma('sp', self.c_hfr[:], self.hfD[cs, :], [dd['hfD']], [self.d_chfr])
                for h in range(4):
                    self.I('dve', 'scalar_tensor_tensor', [dpo, self.d_cw, self.d_chfr], [self.d_ch],
                           hh[:, h * 64:(h + 1) * 64], pov[:, h, 0:64], wv[:, 4 + h:5 + h],
                           self.c_hfr[:, h * 64:(h + 1) * 64], ALU.mult, ALU.add)
                self.I('dve', 'tensor_tensor', [self.d_ch, self.d_cO], [self.d_ch], hh[:], hh[:], self.c_O[:], ALU.mult)
                for h in range(4):
                    self.I('dve', 'bn_stats', [self.d_ch], [self.d_cst], self.c_st[:, h, :], hh[:, h * 64:(h + 1) * 64])
                for h in range(4):
                    self.I('dve', 'bn_aggr', [self.d_cst], [self.d_cst], self.c_mv[:, h, :], self.c_st[:, h, :])
                self.act(self.c_rs[:], self.c_mv[:, :, 1], AF.Sqrt, [self.d_cst], [self.d_cst], bias=LN_EPS)
                self.I('dve', 'reciprocal', [self.d_cst], [self.d_cst], self.c_rs[:], self.c_rs[:])
                for h in range(4):
                    self.I('dve', 'tensor_scalar', [self.d_ch, self.d_cst], [self.d_ch], hh[:, h * 64:(h + 1) * 64],
                           hh[:, h * 64:(h + 1) * 64], self.c_mv[:, h, 0:1], self.c_rs[:, h:h + 1], ALU.subtract, ALU.mult)
                self.I('dve', 'tensor_tensor', [self.d_ch, self.d_cng], [self.d_chn], self.c_hn[:], hh[:], self.c_ng[:], ALU.mult)
                pt, dpt = ps[1], dps[1]
                ptb = pt[:].bitcast(BF16)
                for c2 in range(2):
                    self.tp(ptb[:, c2 * 128:(c2 + 1) * 128], self.c_hn[:, c2 * 128:(c2 + 1) * 128], self.identb[:],
                            [self.d_chn, self.d_ident], [dpt])
                self.I('dve', 'tensor_copy', [dpt], [self.d_chT], self.c_hT[:].rearrange('p a b -> p (a b)'), ptb[:, 0:256])
                for c2 in range(2):
                    self.dma('sp', self.catT[768 + c2 * 128:768 + (c2 + 1) * 128, cs], self.c_hT[:, c2, :], [self.d_chT], [],
                             [dd['catT']])

        vi = 0
        for d in range(2):
            for h in range(4):
                self.I('dve', 'memset', [], [self.d_cC[h]], self.c_C32[:, h, :], 0.0)
                self.I('dve', 'memset', [], [self.d_cC[h]], self.c_Cb[:, h, :], 0.0)
            od = orders[d]
            nxt = local(d, od[0])
            for i_, j in enumerate(od):
                cur = nxt
                if i_ + 1 < len(od):
                    nxt = local(d, od[i_ + 1])
                chain(cur, vi)
                vi += 1

    def ln_affine(self, xt, dx, gbc, bbc, out, dout):
        st, mv, rs = self.e_st, self.e_mv, self.e_rs
        for j in range(2):
            self.I('dve', 'bn_stats', [dx], [self.d_est], st[:, j, :], xt[:, j * 512:(j + 1) * 512])
        self.I('dve', 'bn_aggr', [self.d_est], [self.d_est], mv[:], st[:].rearrange('p a b -> p (a b)'))
        self.act(rs[:, 0:1], mv[:, 1:2], AF.Sqrt, [self.d_est], [self.d_est], bias=LN_EPS)
        self.I('dve', 'reciprocal', [self.d_est], [self.d_est], rs[:, 0:1], rs[:, 0:1])
        self.I('dve', 'scalar_tensor_tensor', [self.d_est], [self.d_est], rs[:, 1:2], mv[:, 0:1], -1.0, rs[:, 0:1],
               ALU.mult, ALU.mult)
        self.act(out[:], xt[:], AF.Identity, [dx, self.d_est], [dout], scale=rs[:, 0:1], bias=rs[:, 1:2])
        self.I('dve', 'tensor_tensor', [dout, self.d_ec], [dout], out[:], out[:], gbc, ALU.mult)
        self.I('dve', 'tensor_tensor', [dout, self.d_ec], [dout], out[:], out[:], bbc, ALU.add)

    def stage_e(self, l, last):
        T, NL = self.T, self.NL
        dd, w = self.dd, self.w
        ntile = T // 128
        SBT = 13
        self.e_st = self.sb('e_st', [128, 2, 6]); self.e_mv = self.sb('e_mv', [128, 2]); self.e_rs = self.sb('e_rs', [128, 2])
        self.d_est = Dep()
        wout = self.sb('e_wout', [128, 8, D], BF16); d_wout = Dep()
        lnc = self.sb('e_lnc', [128, 4, D]); self.d_ec = Dep()
        rw = self.sb('e_rw', [128, 8, 64]); rb = self.sb('e_rb', [128, 64]); d_rw = Dep()
        self.dma('pool', wout[:], w['w_out'][l].rearrange('(kc p) n -> p kc n', p=128), [], [d_wout])
        for i, nm in enumerate(('ln1_g', 'ln1_b', 'ln2_g', 'ln2_b')):
            self.dma('sp', lnc[:, i, :], w[nm][l:l + 1, :].partition_broadcast(128), [], [self.d_ec])
        self.dma('sp', rw[:], w['router_w'][l].rearrange('(kc p) n -> p kc n', p=128), [], [d_rw])
        self.dma('sp', rb[:], w['router_bias'][l:l + 1, :].partition_broadcast(128), [], [d_rw])
        cat = [self.sb('e_cat%d' % i, [128, 8, 128], BF16) for i in range(2)]; d_cat = [Dep(), Dep()]
        xt = [self.sb('e_x%d' % i, [128, D]) for i in range(2)]; d_xt = [Dep(), Dep()]
        tt_ = self.sb('e_t', [128, D]); d_t = Dep()
        x1 = [self.sb('e_x1%d' % i, [128, D]) for i in range(2)]; d_x1 = [Dep(), Dep()]
        fT = self.sb('e_fT', [128, 8, SBT * 128], BF16); d_fT = Dep()
        fT32 = self.sb('e_fT32', [128, 8, 128]); d_fT32 = Dep()
        acc = self.sb('e_acc', [128, SBT, D]); d_acc = [Dep() for _ in range(SBT)]
        G = self.sb('e_G', [128, SBT, 65]); d_G = Dep()
        rt = self.sb('e_rt', [128, 5, 64]); d_rt = Dep()
        t8 = self.sb('e_t8', [128, 9, 8]); d_t8 = Dep()
        sm = self.sb('e_sm', [128, 4, 8]); d_sm = Dep()
        wg = [self.sb('e_wg%d' % i, [128, 8, 512], BF16) for i in range(2)]
        wd = [self.sb('e_wd%d' % i, [128, 2, D], BF16) for i in range(2)]
        d_we = [Dep(), Dep()]
        hs = [self.sb('e_hs%d' % i, [128, 512]) for i in range(2)]; d_hs = [Dep(), Dep()]
        hid = [self.sb('e_hid%d' % i, [128, 2, 512], BF16) for i in range(2)]; d_hid = [Dep(), Dep()]
        self.xres1 = getattr(self, 'xres1', None) or self.dscr('xres1', [T, D])
        dd.setdefault('xres1', Dep())
        self.I('dve', 'memset', [], [d_G], G[:, :, 64:65], 1.0)
        ps, dps = self.ps, self.dps
        ei = 0
        hi_ = 0
        oi = 0
        tiles = list(range(NCTX // 128 if last else 0, ntile))
        n_sb = -(-len(tiles) // SBT)
        per = -(-len(tiles) // n_sb)
        for sb0 in range(0, len(tiles), per):
            blk = tiles[sb0:sb0 + per]
            nb = len(blk)
            for bi, ti in enumerate(blk):
                t0 = ti * 128
                r = 1 if t0 < NCTX else 0
                ct, dct = cat[bi % 2], d_cat[bi % 2]
                xx, dxx = xt[bi % 2], d_xt[bi % 2]
                xo, dxo = x1[bi % 2], d_x1[bi % 2]
                self.dma('sp', ct[:], self.catT[:, t0:t0 + 128].rearrange('(kc p) t -> p kc t', p=128), [dd['catT']], [dct])
                self.dma('sp', xx[:], self.src_rows(l, t0), [dd['xres']], [dxx])
                for half in range(2):
                    pb, dpb = ps[half], dps[half]
                    for kc in range(8):
                        self.mm(pb[:, :], ct[:, kc, :], wout[:, kc, half * 512:(half + 1) * 512], kc == 0, kc == 7,
                                [dct, d_wout], [dpb])
                    self.I('dve', 'tensor_tensor', [dpb, self.d_modbc], [d_t], tt_[:, half * 512:(half + 1) * 512], pb[:, :],
                           self.modbc[:, r, 0, half * 512:(half + 1) * 512], ALU.mult)
                self.I('dve', 'scalar_tensor_tensor', [dxx, d_t], [d_t], tt_[:], xx[:], ALPHA, tt_[:], ALU.mult, ALU.add)
                self.ln_affine(tt_, d_t, lnc[:, 0, :], lnc[:, 1, :], xo, dxo)
                self.dma('sp', self.xres1[t0:t0 + 128, :], xo[:], [dxo], [], [dd['xres1']])
                self.ln_T(xo, dxo, r, 3, 4, fT, d_fT, bi * 128, hT32=fT32)
                pr, dpr = ps[4], dps[4]
                for kc in range(8):
                    self.mm(pr[:, 0:64], fT32[:, kc, :], rw[:, kc, :], kc == 0, kc == 7, [d_fT, d_rw], [dpr])
                self.act(rt[:, 0, :], pr[:, 0:64], AF.Sigmoid, [dpr], [d_rt])
                self.I('dve', 'tensor_tensor', [d_rt, d_rw], [d_rt], rt[:, 1, :], rt[:, 0, :], rb[:], ALU.add)
                for g in range(8):
                    self.I('dve', 'max', [d_rt], [d_t8], t8[:, g, :], rt[:, 1, g * 8:(g + 1) * 8])
                self.I('dve', 'tensor_tensor', [d_t8], [d_sm], sm[:, 0, :], t8[:, 0:8, 0], t8[:, 0:8, 1], ALU.add)
                self.I('dve', 'max', [d_sm], [d_t8], t8[:, 8, :], sm[:, 0, :])
                self.I('dve', 'tensor_scalar', [d_sm, d_t8], [d_sm], sm[:, 1, :], sm[:, 0, :], t8[:, 8, 3:4], None, ALU.is_ge)
                self.I('dve', 'tensor_scalar', [d_sm], [d_sm], sm[:, 2, :], sm[:, 1, :], -1.0, 1e9, ALU.add, ALU.mult)
                for g in range(8):
                    self.I('dve', 'tensor_scalar', [d_rt, d_sm], [d_rt], rt[:, 2, g * 8:(g + 1) * 8], rt[:, 1, g * 8:(g + 1) * 8],
                           sm[:, 1, g:g + 1], sm[:, 2, g:g + 1], ALU.mult, ALU.add)
                self.I('dve', 'max', [d_rt], [d_t8], t8[:, 8, :], rt[:, 2, :])
                self.I('dve', 'tensor_scalar', [d_rt, d_t8], [d_rt], rt[:, 3, :], rt[:, 2, :], t8[:, 8, 7:8], None, ALU.is_ge)
                self.I('dve', 'tensor_tensor', [d_rt], [d_rt], rt[:, 4, :], rt[:, 0, :], rt[:, 3, :], ALU.mult)
                self.I('dve', 'reduce_sum', [d_rt], [d_sm], sm[:, 3, 0:1], rt[:, 4, :], AX.X)
                self.I('dve', 'reciprocal', [d_sm], [d_sm], sm[:, 3, 0:1], sm[:, 3, 0:1])
                self.I('dve', 'tensor_scalar', [d_rt, d_sm], [d_G], G[:, bi, 0:64], rt[:, 4, :], sm[:, 3, 0:1], 2.5,
                       ALU.mult, ALU.mult)
            NB = nb * 128
            items = [(e, c0) for e in range(65) for c0 in range(0, NB, 512)]
            wbuf = {}

            def load_w(e):
                nonlocal ei
                wge, wde, dwe = wg[ei % 2], wd[ei % 2], d_we[ei % 2]
                ei += 1
                if e < 64:
                    srcs = (w['exp_w_gate'][l, e], w['exp_w_up'][l, e], w['exp_w_down'][l, e])
                else:
                    srcs = (w['sh_w_gate'][l], w['sh_w_up'][l], w['sh_w_down'][l])
                self.dma('pool', wge[:, :, 0:256], srcs[0].rearrange('(kc p) n -> p kc n', p=128), [], [dwe])
                self.dma('pool', wge[:, :, 256:512], srcs[1].rearrange('(kc p) n -> p kc n', p=128), [], [dwe])
                self.dma('pool', wde[:], srcs[2].rearrange('(fc p) n -> p fc n', p=128), [], [dwe])
                wbuf[e] = (wge, wde, dwe)

            def gate_up(e, c0):
                nonlocal hi_
                if e not in wbuf:
                    load_w(e)
                wge, wde, dwe = wbuf[e]
                N = min(512, NB - c0)
                hd, dhd = hid[hi_ % 2], d_hid[hi_ % 2]
                hi_ += 1
                for fc in range(2):
                    pg_, dpg_ = ps[2 * fc], dps[2 * fc]
                    pu_, dpu_ = ps[2 * fc + 1], dps[2 * fc + 1]
                    for kc in range(8):
                        self.mm(pg_[:, :N], wge[:, kc, fc * 128:(fc + 1) * 128], fT[:, kc, c0:c0 + N], kc == 0, kc == 7,
                                [dwe, d_fT], [dpg_])
                    for kc in range(8):
                        self.mm(pu_[:, :N], wge[:, kc, 256 + fc * 128:256 + (fc + 1) * 128], fT[:, kc, c0:c0 + N], kc == 0,
                                kc == 7, [dwe, d_fT], [dpu_])
                    hh, dhh = hs[fc], d_hs[fc]
                    self.act(hh[:, :N], pg_[:, :N], AF.Silu, [dpg_], [dhh])
                    self.I('dve', 'tensor_tensor', [dhh, dpu_], [dhd], hd[:, fc, :N], hh[:, :N], pu_[:, :N], ALU.mult)
                return hd, dhd

            def down(e, c0, hd, dhd):
                nonlocal oi
                wge, wde, dwe = wbuf[e]
                N = min(512, NB - c0)
                for s_ in range(N // 128):
                    bi = (c0 + s_ * 128) // 128
                    for half in range(2):
                        po_, dpo_ = ps[4 + oi % 4], dps[4 + oi % 4]
                        oi += 1
                        for fc in range(2):
                            self.mm(po_[:, :], hd[:, fc, s_ * 128:(s_ + 1) * 128], wde[:, fc, half * 512:(half + 1) * 512],
                                    fc == 0, fc == 1, [dhd, dwe], [dpo_])
                        av = acc[:, bi, half * 512:(half + 1) * 512]
                        if e == 0:
                            self.I('dve', 'tensor_scalar', [dpo_, d_G], [d_acc[bi]], av, po_[:, :], G[:, bi, e:e + 1], None,
                                   ALU.mult)
                        else:
                            self.I('dve', 'scalar_tensor_tensor', [dpo_, d_G, d_acc[bi]], [d_acc[bi]], av, po_[:, :],
                                   G[:, bi, e:e + 1], av, ALU.mult, ALU.add)

            prev = None
            for (e, c0) in items:
                hd, dhd = gate_up(e, c0)
                if prev is not None:
                    down(*prev)
                prev = (e, c0, hd, dhd)
                if c0 == 0 and e + 1 < 65 and (e + 1) not in wbuf:
                    load_w(e + 1)
                    wbuf.pop(e - 1, None)
            down(*prev)
            for bi, ti in enumerate(blk):
                t0 = ti * 128
                r = 1 if t0 < NCTX else 0
                xo, dxo = x1[bi % 2], d_x1[bi % 2]
                self.dma('sp', xo[:], self.xres1[t0:t0 + 128, :], [dd['xres1']], [dxo])
                self.I('dve', 'tensor_tensor', [d_acc[bi], self.d_modbc], [d_acc[bi]], acc[:, bi, :], acc[:, bi, :],
                       self.modbc[:, r, 1, :], ALU.mult)
                self.I('dve', 'scalar_tensor_tensor', [dxo, d_acc[bi]], [d_acc[bi]], acc[:, bi, :], xo[:], ALPHA, acc[:, bi, :],
                       ALU.mult, ALU.add)
                self.ln_affine(acc[:, bi, :], d_acc[bi], lnc[:, 2, :], lnc[:, 3, :], xo, dxo)
                if not last:
                    self.dma('sp', self.xres[t0:t0 + 128, :], xo[:], [dxo], [], [dd['xres']])
                elif t0 >= NCTX:
                    self.outs.append(self.dma('sp', self.out[t0 - NCTX:t0 - NCTX + 128, :], xo[:], [dxo], [], [dd['out']]))


    def stage_d(self, l):
        T, NL = self.T, self.NL
        NJ, NC8 = T // 8, NCTX // 8
        NL8 = NJ - NC8
        dd, w = self.dd, self.w
        dt_ = Dep()
        ops = {'n': 0}

        def V(eng, method, *a_, **kw):
            return self.I(eng, method, [dt_], [dt_], *a_, **kw)

        def A(out, in_, func, **kw):
            return self.act(out, in_, func, [dt_], [dt_], **kw)

        def tl(name, shape):
            return self.sb('d_' + name, shape)
        t1 = tl('t1', [128, 512]); t2 = tl('t2', [128, 512])

        def cm(or_, oi_, ar, ai, br, bi, n, shp=None):
            def v(t):
                x = t[:, 0:n]
                return x if shp is None else x.rearrange(shp[0], **shp[1])
            V('dve', 'tensor_tensor', v(t1), ar, br, ALU.mult)
            V('dve', 'tensor_tensor', v(t2), ai, bi, ALU.mult)
            V('dve', 'tensor_tensor', v(t1), v(t1), v(t2), ALU.subtract)
            V('dve', 'tensor_tensor', v(t2), ar, bi, ALU.mult)
            V('dve', 'tensor_tensor', oi_, ai, br, ALU.mult)
            V('dve', 'tensor_tensor', oi_, oi_, v(t2), ALU.add)
            V('dve', 'tensor_copy', or_, v(t1))

        lr = tl('lr', [128, 32]); li = tl('li', [128, 32]); dl = tl('dl', [128, 32])
        for hf in range(2):
            ps_ = slice(hf * 64, (hf + 1) * 64)
            self.dma('sp', lr[ps_, :], w['s5_lambda_re'][l].rearrange('d g p -> p (d g)'), [], [dt_], allow_slow_non_contiguous=True)
            self.dma('sp', li[ps_, :], w['s5_lambda_im'][l].rearrange('d g p -> p (d g)'), [], [dt_], allow_slow_non_contiguous=True)
        self.dma('sp', dl[:], w['s5_log_step'][l:l + 1].rearrange('o d g -> o (d g)').partition_broadcast(128), [], [dt_])
        A(dl[:], dl[:], AF.Exp)
        xr = tl('xr', [128, 32]); xi = tl('xi', [128, 32])
        V('dve', 'tensor_tensor', xr[:], lr[:], dl[:], ALU.mult)
        V('dve', 'tensor_tensor', xi[:], li[:], dl[:], ALU.mult)
        PWr = tl('PWr', [128, 16, 32]); PWi = tl('PWi', [128, 16, 32])
        NPr = tl('NPr', [128, 8, 32]); NPi = tl('NPi', [128, 8, 32])
        KK = max(1, int(math.ceil(math.log2(NJ))))
        AKr = tl('AKr', [128, KK, 32]); AKi = tl('AKi', [128, KK, 32])
        m16 = tl('m16', [128, 32]); c16 = tl('c16', [128, 32]); s16 = tl('s16', [128, 32])
        ar = tl('ar', [128, 32]); ai = tl('ai', [128, 32])
        A(m16[:], xr[:], AF.Exp, scale=1.0 / 16)
        A(c16[:], xi[:], AF.Sin, scale=1.0 / 16, bias=math.pi / 2)
        A(s16[:], xi[:], AF.Sin, scale=1.0 / 16)
        V('dve', 'tensor_tensor', ar[:], m16[:], c16[:], ALU.mult)
        V('dve', 'tensor_tensor', ai[:], m16[:], s16[:], ALU.mult)
        for _ in range(4):
            cm(ar[:], ai[:], ar[:], ai[:], ar[:], ai[:], 32)
        V('dve', 'memset', PWr[:, 0, :], 1.0); V('dve', 'memset', PWi[:, 0, :], 0.0)
        for s_ in range(1, 16):
            cm(PWr[:, s_, :], PWi[:, s_, :], PWr[:, s_ - 1, :], PWi[:, s_ - 1, :], ar[:], ai[:], 32)
        iar = tl('iar', [128, 32]); iai = tl('iai', [128, 32]); n2 = tl('n2', [128, 32])
        V('dve', 'tensor_tensor', n2[:], ar[:], ar[:], ALU.mult)
        V('dve', 'tensor_tensor', iar[:], ai[:], ai[:], ALU.mult)
        V('dve', 'tensor_tensor', n2[:], n2[:], iar[:], ALU.add)
        V('dve', 'reciprocal', n2[:], n2[:])
        V('dve', 'tensor_tensor', iar[:], ar[:], n2[:], ALU.mult)
        V('dve', 'scalar_tensor_tensor', iai[:], ai[:], -1.0, n2[:], ALU.mult, ALU.mult)
        V('dve', 'memset', NPr[:, 0, :], 1.0); V('dve', 'memset', NPi[:, 0, :], 0.0)
        for s_ in range(1, 8):
            cm(NPr[:, s_, :], NPi[:, s_, :], NPr[:, s_ - 1, :], NPi[:, s_ - 1, :], iar[:], iai[:], 32)
        V('dve', 'tensor_copy', AKr[:, 0, :], PWr[:, 8, :]); V('dve', 'tensor_copy', AKi[:, 0, :], PWi[:, 8, :])
        for k in range(1, KK):
            cm(AKr[:, k, :], AKi[:, k, :], AKr[:, k - 1, :], AKi[:, k - 1, :], AKr[:, k - 1, :], AKi[:, k - 1, :], 32)
        V('dve', 'tensor_scalar', AKi[64:128, :, :], AKi[64:128, :, :], -1.0, None, ALU.mult)
        cr = tl('cr', [128, 32]); ci = tl('ci', [128, 32]); am = tl('am', [128, 32]); dn = tl('dn', [128, 32])
        V('dve', 'tensor_scalar', am[:], ar[:], -1.0, None, ALU.add)
        V('dve', 'tensor_tensor', cr[:], am[:], lr[:], ALU.mult)
        V('dve', 'tensor_tensor', dn[:], ai[:], li[:], ALU.mult)
        V('dve', 'tensor_tensor', cr[:], cr[:], dn[:], ALU.add)
        V('dve', 'tensor_tensor', ci[:], ai[:], lr[:], ALU.mult)
        V('dve', 'tensor_tensor', dn[:], am[:], li[:], ALU.mult)
        V('dve', 'tensor_tensor', ci[:], ci[:], dn[:], ALU.subtract)
        V('dve', 'tensor_tensor', dn[:], lr[:], lr[:], ALU.mult)
        V('dve', 'tensor_tensor', am[:], li[:], li[:], ALU.mult)
        V('dve', 'tensor_tensor', dn[:], dn[:], am[:], ALU.add)
        V('dve', 'reciprocal', dn[:], dn[:])
        V('dve', 'tensor_tensor', cr[:], cr[:], dn[:], ALU.mult)
        V('dve', 'tensor_tensor', ci[:], ci[:], dn[:], ALU.mult)
        Br = tl('Br', [128, 32, 16]); Bi = tl('Bi', [128, 32, 16])
        for hf in range(2):
            ps_ = slice(hf * 64, (hf + 1) * 64)
            self.dma('sp', Br[ps_], w['s5_b_re'][l].rearrange('d g p c -> p (d g) c'), [], [dt_])
            self.dma('sp', Bi[ps_], w['s5_b_im'][l].rearrange('d g p c -> p (d g) c'), [], [dt_])
        shp3 = ('p (a b) -> p a b', {'a': 32})
        bc3 = lambda t: t.unsqueeze(2).to_broadcast([128, 32, 16])
        cm(Br[:], Bi[:], bc3(cr[:]), bc3(ci[:]), Br[:], Bi[:], 512, shp3)
        Cr = tl('Cr', [128, 32, 16]); Ci = tl('Ci', [128, 32, 16])
        cn = tl('cn', [128, 128])
        idn = self.ident
        for (nm, Ct) in (('s5_c_re', Cr), ('s5_c_im', Ci)):
            for d in range(2):
                for gh in range(2):
                    src = w[nm][l, d, gh * 8:(gh + 1) * 8].rearrange('g c p -> (g c) p')
                    self.dma('sp', cn[:, 0:64], src, [], [dt_])
                    self.dma('sp', cn[:, 64:128], src, [], [dt_])
                    pb, dpb = self.ps[0], self.dps[0]
                    self.tp(pb[:, 0:128], cn[:], idn[:], [dt_, self.d_ident], [dpb])
                    o0 = d * 16 + gh * 8
                    self.I('dve', 'tensor_copy', [dpb, dt_], [dt_], Ct[:, o0:o0 + 8, :].rearrange('p a b -> p (a b)'), pb[:, 0:128])
        XS = tl('XS', [128, 32, 8, 16]); XSA = tl('XSA', [128, 32, 8, 16]); YS = tl('YS', [128, 32, 8, 16])
        bc2 = lambda t: t.unsqueeze(2).to_broadcast([t.shape[0], 16, 16])
        h0, h1 = slice(0, 64), slice(64, 128)
        t1v = t1[:, 0:256].rearrange('p (a b) -> p a b', a=16)
        t2v = t2[:, 0:256].rearrange('p (a b) -> p a b', a=16)

        def cmh(out, pr, pi_, br, bi, neg):
            V('dve', 'tensor_tensor', t1v[h0], bc2(pr[h0]), br[h0], ALU.mult)
            V('dve', 'tensor_tensor', t2v[h0], bc2(pi_[h0]), bi[h0], ALU.mult)
            V('dve', 'tensor_tensor', out[h0], t1v[h0], t2v[h0], ALU.subtract)
            V('dve', 'tensor_tensor', t1v[h1], bc2(pr[h1]), bi[h1], ALU.mult)
            V('dve', 'tensor_tensor', t2v[h1], bc2(pi_[h1]), br[h1], ALU.mult)
            if neg:
                V('dve', 'scalar_tensor_tensor', out[h1], t1v[h1], -1.0, t2v[h1], ALU.mult, ALU.subtract)
            else:
                V('dve', 'tensor_tensor', out[h1], t1v[h1], t2v[h1], ALU.add)
        for d in range(2):
            dc = slice(d * 16, (d + 1) * 16)
            for s_ in range(8):
                if d == 0:
                    pX = (NPr[:, s_, dc], NPi[:, s_, dc]); pXA = (PWr[:, 8 - s_, dc], PWi[:, 8 - s_, dc])
                    pY = (PWr[:, s_, dc], PWi[:, s_, dc])
                else:
                    pX = (PWr[:, s_, dc], PWi[:, s_, dc]); pXA = (PWr[:, 8 + s_, dc], PWi[:, 8 + s_, dc])
                    pY = (NPr[:, s_, dc], NPi[:, s_, dc])
                cmh(XS[:, dc, s_, :], pX[0], pX[1], Br[:, dc, :], Bi[:, dc, :], False)
                cmh(XSA[:, dc, s_, :], pXA[0], pXA[1], Br[:, dc, :], Bi[:, dc, :], False)
                cmh(YS[:, dc, s_, :], pY[0], pY[1], Cr[:, dc, :], Ci[:, dc, :], True)
        YSb = self.sb('d_YSb', [128, 32, 128], BF16)
        V('dve', 'tensor_copy', YSb[:], YS[:].rearrange('p a b c -> p a (b c)'))
        XTAb = self.sb('d_XTAb', [128, 32, 128], BF16)
        KMb = self.sb('d_KMb', [128, 16, 128], BF16)
        cst = tl('cst', [128, 3, 128])
        self.dma('sp', cst[:, 0, :], self.k['k_sw'], [], [dt_])
        self.dma('sp', cst[:, 1, :], self.k['k_m8f'], [], [dt_])
        self.dma('sp', cst[:, 2, :], self.k['k_m8b'], [], [dt_])
        dS = tl('dS', [128, 16])
        for s_ in range(8):
            self.dma('sp', dS[16 * s_:16 * s_ + 16, :], w['s5_d'][l].rearrange('(g c) -> c g', c=16), [], [dt_],
                     allow_slow_non_contiguous=True)
        km = tl('km', [128, 128])
        for dg in range(32):
            pb, dpb = self.ps[dg % 2], self.dps[dg % 2]
            self.tp(pb[:, 0:128], XSA[:, dg, :, :].rearrange('p b c -> p (b c)'), idn[:], [dt_, self.d_ident], [dpb])
            self.I('dve', 'tensor_copy', [dpb, dt_], [dt_], XTAb[:, dg, :], pb[:, 0:128])
        for g in range(16):
            pf, dpf = self.ps[2], self.dps[2]
            pb_, dpb_ = self.ps[3], self.dps[3]
            self.mm(pf[:, 0:128], XS[:, g, :, :].rearrange('p b c -> p (b c)'), YS[:, g, :, :].rearrange('p b c -> p (b c)'),
                    True, True, [dt_], [dpf])
            self.mm(pb_[:, 0:128], XS[:, 16 + g, :, :].rearrange('p b c -> p (b c)'),
                    YS[:, 16 + g, :, :].rearrange('p b c -> p (b c)'), True, True, [dt_], [dpb_])
            self.I('dve', 'tensor_tensor', [dpf, dt_], [dt_], km[:], pf[:, 0:128], cst[:, 1, :], ALU.mult)
            self.I('dve', 'tensor_tensor', [dpb_, dt_], [dt_], t1[:, 0:128], pb_[:, 0:128], cst[:, 2, :], ALU.mult)
            V('dve', 'tensor_tensor', km[:], km[:], t1[:, 0:128], ALU.add)
            V('dve', 'scalar_tensor_tensor', KMb[:, g, :], idn[:], dS[:, g:g + 1], km[:], ALU.mult, ALU.add)
        U = [self.sb('d_U%d' % i, [128, NJ], BF16) for i in range(2)]; dU = [Dep(), Dep()]
        P = [self.sb('d_P%d' % i, [128, NJ]) for i in range(2)]; dP = [Dep(), Dep()]
        Sx = [self.sb('d_S%d' % i, [128, NJ], BF16) for i in range(2)]; dSx = [Dep(), Dep()]
        Rm = [self.sb('d_R%d' % i, [128, 128]) for i in range(2)]; dR = [Dep(), Dep()]
        yg = [self.sb('d_yg%d' % i, [128, NJ]) for i in range(2)]; dyg = [Dep(), Dep()]
        blocks = [(c0, min(512, NJ - c0)) for c0 in range(0, NJ, 512)]
        ps, dps = self.ps, self.dps
        ri = 0
        for g in range(16):
            Ug, dUg = U[g % 2], dU[g % 2]
            for s_ in range(8):
                self.dma('sp', Ug[16 * s_:16 * s_ + 16, :], self.zTu[16 * g:16 * g + 16, s_, :], [dd['zTu']], [dUg])
            for d in range(2):
                dg = d * 16 + g
                Pd, dPd = P[d], dP[d]
                for (c0, n) in blocks:
                    pb, dpb = ps[(c0 // 512) % 3], dps[(c0 // 512) % 3]
                    self.mm(pb[:, :n], XTAb[:, dg, :], Ug[:, c0:c0 + n], True, True, [dt_, dUg], [dpb])
                    if d == 0:
                        self.act(Pd[:, c0:c0 + n], pb[:, :n], AF.Copy, [dpb], [dPd])
                    else:
                        lo, hi = c0, c0 + n
                        if lo < NC8:
                            m = min(hi, NC8) - lo
                            self.act(Pd[:, NL8 + lo:NL8 + lo + m], pb[:, 0:m], AF.Copy, [dpb], [dPd])
                        if hi > NC8:
                            a0 = max(lo, NC8)
                            self.act(Pd[:, a0 - NC8:hi - NC8], pb[:, a0 - lo:n], AF.Copy, [dpb], [dPd])
                for k in range(KK):
                    sh = 1 << k
                    if sh >= NJ:
                        break
                    R_, dR_ = Rm[ri % 2], dR[ri % 2]
                    ri += 1
                    self.I('dve', 'tensor_scalar', [dt_], [dR_], R_[:], idn[:], AKr[:, k, dg:dg + 1], None, ALU.mult)
                    self.I('dve', 'scalar_tensor_tensor', [dt_, dR_], [dR_], R_[:], cst[:, 0, :], AKi[:, k, dg:dg + 1], R_[:],
                           ALU.mult, ALU.add)
                    n_tot = NJ - sh
                    segs = [(c0, min(512, n_tot - c0)) for c0 in range(0, n_tot, 512)]
                    pend = []
                    for bi_, (c0, n) in enumerate(segs):
                        pb, dpb = ps[bi_ % 3], dps[bi_ % 3]
                        src0 = c0 if d == 0 else c0 + sh
                        self.mm(pb[:, :n], R_[:], Pd[:, src0:src0 + n], True, True, [dR_, dPd], [dpb])
                        pend.append((pb, dpb, c0, n))
                    for (pb, dpb, c0, n) in pend:
                        dst0 = c0 + sh if d == 0 else c0
                        self.I('dve', 'tensor_tensor', [dpb, dPd], [dPd], Pd[:, dst0:dst0 + n], Pd[:, dst0:dst0 + n], pb[:, :n],
                               ALU.add)
                Sd, dSd = Sx[d], dSx[d]
                if d == 0:
                    self.I('dve', 'memset', [], [dSd], Sd[:, 0:1], 0.0)
                    self.I('dve', 'tensor_copy', [dPd], [dSd], Sd[:, 1:NJ], Pd[:, 0:NJ - 1])
                else:
                    self.I('dve', 'tensor_copy', [dPd], [dSd], Sd[:, NC8:NJ], Pd[:, 1:NL8 + 1])
                    self.I('dve', 'tensor_copy', [dPd], [dSd], Sd[:, 0:NC8 - 1], Pd[:, NL8 + 1:NJ])
                    self.I('dve', 'memset', [], [dSd], Sd[:, NC8 - 1:NC8], 0.0)
            ygg, dygg = yg[g % 2], dyg[g % 2]
            for (c0, n) in blocks:
                pb, dpb = ps[4 + (c0 // 512) % 3], dps[4 + (c0 // 512) % 3]
                self.mm(pb[:, :n], KMb[:, g, :], Ug[:, c0:c0 + n], True, False, [dt_, dUg], [dpb])
                self.mm(pb[:, :n], YSb[:, g, :], Sx[0][:, c0:c0 + n], False, False, [dt_, dSx[0]], [dpb])
                self.mm(pb[:, :n], YSb[:, 16 + g, :], Sx[1][:, c0:c0 + n], False, True, [dt_, dSx[1]], [dpb])
                self.act(ygg[:, c0:c0 + n], pb[:, :n], AF.Copy, [dpb], [dygg])
            for t_ in range(8):
                self.dma('sp', self.y8[16 * g:16 * g + 16, t_, :], ygg[16 * t_:16 * t_ + 16, :], [dygg], [], [dd['y8']])
        gw = self.sb('d_gw', [128, 2, 256], BF16); gbv = tl('gbv', [128, 2]); dgw = Dep()
        self.dma('pool', gw[:], w['s5_glu_w'][l].rearrange('(kc p) n -> p kc n', p=128), [], [dgw])
        for c in range(2):
            self.dma('sp', gbv[:, c:c + 1], w['s5_glu_b'][l, c * 128:(c + 1) * 128].rearrange('(p o) -> p o', o=1), [], [dgw],
                     allow_slow_non_contiguous=True)
        CH = 512
        yv = [self.sb('d_yv%d' % i, [128, CH]) for i in range(2)]; dyv = [Dep(), Dep()]
        ga = self.sb('d_ga', [128, CH]); dga = Dep()
        gT = self.sb('d_gT', [128, 2, CH], BF16); gF = self.sb('d_gF', [128, 2, CH]); dgT = Dep()
        ob = [self.sb('d_ob%d' % i, [128, CH], BF16) for i in range(2)]; dob = [Dep(), Dep()]
        sg = self.sb('d_sg', [128, CH]); dsg = Dep()
        K2 = 2.0 * math.sqrt(2.0 / math.pi)
        oi = 0
        for j0 in range(0, NJ, 64):
            nj = min(64, NJ - j0)
            n = nj * 8
            for c in range(2):
                yy, dyy = yv[c], dyv[c]
                self.dma('sp', yy[:, :n].rearrange('p (s j) -> p s j', s=8), self.y8[c * 128:(c + 1) * 128, :, j0:j0 + nj],
                         [dd['y8']], [dyy])
                self.I('dve', 'tensor_tensor', [dyy], [dga], ga[:, :n], yy[:, :n], yy[:, :n], ALU.mult)
                self.I('dve', 'tensor_scalar', [dga], [dga], ga[:, :n], ga[:, :n], 0.044715, 1.0, ALU.mult, ALU.add)
                self.I('dve', 'tensor_tensor', [dga, dyy], [dga], ga[:, :n], ga[:, :n], yy[:, :n], ALU.mult)
                self.act(ga[:, :n], ga[:, :n], AF.Sigmoid, [dga], [dga], scale=K2)
                self.I('dve', 'tensor_tensor', [dga, dyy], [dgT], gF[:, c, :n], ga[:, :n], yy[:, :n], ALU.mult)
                self.I('dve', 'tensor_copy', [dgT], [dgT], gT[:, c, :n], gF[:, c, :n])
            for c in range(2):
                pb, dpb = ps[c], dps[c]
                for kc in range(2):
                    self.mm(pb[:, :n], gw[:, kc, c * 128:(c + 1) * 128], gT[:, kc, :n], kc == 0, kc == 1, [dgw, dgT], [dpb])
                self.act(sg[:, :n], pb[:, :n], AF.Sigmoid, [dpb, dgw], [dsg], bias=gbv[:, c:c + 1])
                o_, do_ = ob[oi % 2], dob[oi % 2]
                oi += 1
                self.I('dve', 'tensor_tensor', [dsg, dgT], [do_], o_[:, :n].rearrange('p (j s) -> p s j', s=8),
                       sg[:, :n].rearrange('p (s j) -> p s j', s=8), gF[:, c, :n].rearrange('p (s j) -> p s j', s=8), ALU.mult)
                self.dma('sp', self.catT[c * 128:(c + 1) * 128, 8 * j0:8 * j0 + n], o_[:, :n], [do_], [], [dd['catT']])


def make_inmap(inputs, b, NL):
    m = {}
    for k in inputs.keys() if hasattr(inputs, 'keys') else inputs.files:
        if k in ('x', 'c', 'ctx', 'c_ctx'):
            continue
        a = np.asarray(inputs[k])
        if k == 'ml_gate_b':
            a = a.reshape(a.shape[0], 16)
        m[k] = np.ascontiguousarray(a, dtype=np.float32)
    m['x'] = np.ascontiguousarray(np.asarray(inputs['x'])[b, :NL], dtype=np.float32)
    m['ctx'] = np.ascontiguousarray(np.asarray(inputs['ctx'])[b], dtype=np.float32)
    m['cvec'] = np.ascontiguousarray(np.stack([np.asarray(inputs['c'])[b], np.asarray(inputs['c_ctx'])]), dtype=np.float32)
    m.update(host_consts(NL))
    return m


def build_program(NL, debug=()):
    b = B(NL, debug=debug)
    b.setup()
    for l in range(DEPTH):
        b.stage_begin(); b.stage_mod(l)
        b.stage_begin(); b.stage_a_weights(l); b.stage_a(l)
        b.stage_begin(); b.stage_b(l)
        b.stage_begin(); b.stage_c(l)
        b.stage_begin(); b.stage_d(l)
        b.stage_begin(); b.stage_e(l, l == DEPTH - 1)
    b.finish(b.outs)
    return b


def kernel(**inputs):
    NL = inputs['x'].shape[1]
    nb = inputs['x'].shape[0]
    b = build_program(NL)
    in_maps = [make_inmap(inputs, i, NL) for i in range(nb)]
    res = run_bass_kernel_spmd(b.nc, in_maps, core_ids=list(range(nb)))
    return np.stack([np.asarray(r['out'], dtype=np.float32) for r in res.results], axis=0)
```
